# Optimizing a Trainium2 kernel written in Bass

```python
import jax, jax.numpy as jnp
from jax import lax
import numpy as np

D_MODEL = 1024
BATCH = 8
SEQ = 4096
DEPTH = 1

ATT_HEAD_DIM = 128
ATT_GROUPS = ((128, 1), (512, 4), (2048, 16))
ATT_N_GROUPS = 3
ATT_KV_HEADS = 4
ATT_Q_HEADS = ATT_N_GROUPS * ATT_KV_HEADS
ATT_Q_WIDTH = ATT_Q_HEADS * ATT_HEAD_DIM
ATT_KV_WIDTH = ATT_KV_HEADS * ATT_HEAD_DIM
ATT_BLOCK = 128
ROPE_THETA = 10000.0
M_HEADS = 4
M_HEAD_DIM = 256
M_WIDTH = M_HEADS * M_HEAD_DIM
M_CONV = 4
M_CHUNK = 128
IN_WIDTH = ATT_Q_WIDTH + 3 * ATT_KV_WIDTH + 3 * M_WIDTH + 2 * D_MODEL
LN_EPS = 1e-5

kernel_name = 'hybrid_dilated_attn_mlstm_gated_merge'


def _layer_norm(x):
    xf = x.astype(jnp.float32)
    mu = jnp.mean(xf, axis=-1, keepdims=True)
    var = jnp.mean(jnp.square(xf - mu), axis=-1, keepdims=True)
    return (xf - mu) * lax.rsqrt(var + LN_EPS)


def _rope(x, pos):
    half = x.shape[-1] // 2
    inv = jnp.power(ROPE_THETA, -jnp.arange(half, dtype=jnp.float32) / half)
    ang = pos.astype(jnp.float32)[..., None] * inv
    cos = jnp.cos(ang)[:, :, None, :]
    sin = jnp.sin(ang)[:, :, None, :]
    x1, x2 = x[..., :half], x[..., half:]
    return jnp.concatenate([x1 * cos - x2 * sin, x2 * cos + x1 * sin], axis=-1)


def _dilated_window_attention(q, k, v, dilation, n_steps):
    B, S, H, E = q.shape
    span = dilation * ATT_BLOCK
    s_pad = -(-S // span) * span
    pad = ((0, 0), (0, s_pad - S), (0, 0), (0, 0))
    q, k, v = jnp.pad(q, pad), jnp.pad(k, pad), jnp.pad(v, pad)
    nb = s_pad // span
    shape = (B, nb, ATT_BLOCK, dilation, H, E)
    qb, kb, vb = q.reshape(shape), k.reshape(shape), v.reshape(shape)

    def with_prev(a):
        prev = jnp.pad(a[:, :-1], ((0, 0), (1, 0), (0, 0), (0, 0), (0, 0), (0, 0)))
        return jnp.concatenate([prev, a], axis=2)

    kk, vv = with_prev(kb), with_prev(vb)
    s = jnp.einsum('bnidhe,bnjdhe->bndhij', qb, kk)
    i = jnp.arange(ATT_BLOCK)[:, None]
    j = jnp.arange(2 * ATT_BLOCK)[None, :]
    dist = ATT_BLOCK + i - j
    first = (jnp.arange(nb) == 0)[:, None, None]
    valid = (dist >= 0) & (dist <= n_steps) & ~(first & (j < ATT_BLOCK))
    s = jnp.where(valid[None, :, None, None], s, -jnp.inf)
    m = jnp.max(s, axis=-1, keepdims=True)
    p = jnp.exp(s - m)
    den = jnp.sum(p, axis=-1)
    o = jnp.einsum('bndhij,bnjdhe->bnidhe', p, vv) / jnp.transpose(den, (0, 1, 4, 2, 3))[..., None]
    lse = jnp.transpose(m[..., 0] + jnp.log(den), (0, 1, 4, 2, 3))
    o = o.reshape(B, s_pad, H, E)[:, :S]
    lse = lse.reshape(B, s_pad, H)[:, :S]
    return o, lse


def _causal_conv(x, w, b):
    C = x.shape[-1]
    y = lax.conv_general_dilated(
        x.astype(jnp.float32), w.astype(jnp.float32)[:, None, :], window_strides=(1,),
        padding=[(M_CONV - 1, 0)], dimension_numbers=('NWC', 'WIO', 'NWC'),
        feature_group_count=C)
    return y + b.astype(jnp.float32)


def _mlstm_chunkwise(q, k, v, log_i, log_f):
    B, S, H, E = q.shape
    nc = S // M_CHUNK

    def chunks(a):
        return a.reshape(B, nc, M_CHUNK, H, E).transpose(0, 3, 1, 2, 4)

    def gchunks(a):
        return a.reshape(B, nc, M_CHUNK, H).transpose(0, 3, 1, 2)

    q, k, v = chunks(q), chunks(k), chunks(v)
    li, lf = gchunks(log_i), gchunks(log_f)
    b = jnp.cumsum(lf, axis=-1)
    b_last = b[..., -1]
    w_src = b_last[..., None] - b + li
    a = jnp.max(w_src, axis=-1)
    e_src = jnp.exp(w_src - a[..., None])
    c_loc = jnp.einsum('bhcs,bhcsd,bhcse->bhcde', e_src, v, k)
    n_loc = jnp.einsum('bhcs,bhcse->bhce', e_src, k)

    def step(carry, inp):
        c_st, n_st, m_st = carry
        c_l, n_l, a_c, bl, q_c = inp
        num_inter = jnp.einsum('bhde,bhte->bhtd', c_st, q_c)
        den_inter = jnp.einsum('bhe,bhte->bht', n_st, q_c)
        m_new = jnp.maximum(bl + m_st, a_c)
        decay = jnp.exp(bl + m_st - m_new)
        gain = jnp.exp(a_c - m_new)
        c_st = decay[..., None, None] * c_st + gain[..., None, None] * c_l
        n_st = decay[..., None] * n_st + gain[..., None] * n_l
        return (c_st, n_st, m_new), (num_inter, den_inter, m_st)

    init = (jnp.zeros((B, H, E, E), jnp.float32), jnp.zeros((B, H, E), jnp.float32),
            jnp.zeros((B, H), jnp.float32))
    xs = (jnp.moveaxis(c_loc, 2, 0), jnp.moveaxis(n_loc, 2, 0), jnp.moveaxis(a, 2, 0),
          jnp.moveaxis(b_last, 2, 0), jnp.moveaxis(q, 2, 0))
    _, (num_inter, den_inter, m_prev) = lax.scan(step, init, xs)
    num_inter = jnp.moveaxis(num_inter, 0, 2)
    den_inter = jnp.moveaxis(den_inter, 0, 2)
    m_prev = jnp.moveaxis(m_prev, 0, 2)

    causal = jnp.tril(jnp.ones((M_CHUNK, M_CHUNK), dtype=bool))
    dmat = jnp.where(causal, b[..., :, None] - b[..., None, :] + li[..., None, :], -jnp.inf)
    g = b + m_prev[..., None]
    m_t = jnp.maximum(g, jnp.max(dmat, axis=-1))
    smat = jnp.einsum('bhcte,bhcse->bhcts', q, k) * jnp.exp(dmat - m_t[..., None])
    w_inter = jnp.exp(g - m_t)
    num = w_inter[..., None] * num_inter + jnp.einsum('bhcts,bhcsd->bhctd', smat, v)
    den = w_inter * den_inter + jnp.sum(smat, axis=-1)
    h = num / jnp.maximum(jnp.abs(den), jnp.exp(-m_t))[..., None]
    return h.transpose(0, 2, 3, 1, 4).reshape(B, S, H, E)


def _hybrid_layer(x, c, positions, w_ada, b_ada, w_in, conv_w, conv_b, w_qm, w_km, w_vm,
                  w_if, b_if, mh_norm_w, skip_m, w_pa, w_pm, w_out, ln_g, ln_b):
    B, S, _ = x.shape
    dt = x.dtype
    f32 = jnp.float32
    alpha = (2.0 * DEPTH) ** 0.25
    ada = (jax.nn.silu(c) @ w_ada + b_ada).astype(f32)
    shift, scale, gate = jnp.split(ada, 3, axis=-1)
    h = (_layer_norm(x) * (1.0 + scale[:, None]) + shift[:, None]).astype(dt)

    proj = h @ w_in
    sizes = [ATT_Q_WIDTH, ATT_KV_WIDTH, ATT_KV_WIDTH, ATT_KV_WIDTH, M_WIDTH, M_WIDTH, M_WIDTH, D_MODEL]
    offs = [int(o) for o in np.cumsum(sizes)]
    q_a, k_a, v_a, z_a, x_m, z_m, o_m, g_a, g_m = jnp.split(proj, offs, axis=-1)

    q = _rope(q_a.reshape(B, S, ATT_Q_HEADS, ATT_HEAD_DIM).astype(f32), positions) * (ATT_HEAD_DIM ** -0.5)
    q = q.reshape(B, S, ATT_N_GROUPS, ATT_KV_HEADS, ATT_HEAD_DIM)
    k = _rope(k_a.reshape(B, S, ATT_KV_HEADS, ATT_HEAD_DIM).astype(f32), positions)
    v = v_a.reshape(B, S, ATT_KV_HEADS, ATT_HEAD_DIM).astype(f32)
    outs, lses = [], []
    for g_idx, (window, dil) in enumerate(ATT_GROUPS):
        o_g, l_g = _dilated_window_attention(q[:, :, g_idx], k, v, dil, window // dil)
        outs.append(o_g)
        lses.append(l_g)
    wgt = jax.nn.softmax(jnp.stack(lses, axis=0), axis=0)
    o_att = jnp.sum(wgt[..., None] * jnp.stack(outs, axis=0), axis=0).reshape(B, S, ATT_KV_WIDTH)
    y_att = (o_att * jax.nn.silu(z_a.astype(f32))).astype(dt) @ w_pa

    xc = jax.nn.silu(_causal_conv(x_m, conv_w, conv_b))
    xch = xc.reshape(B, S, M_HEADS, M_HEAD_DIM)
    qm = jnp.einsum('bshd,hde->bshe', xch, w_qm.astype(f32))
    km = jnp.einsum('bshd,hde->bshe', xch, w_km.astype(f32)) * (M_HEAD_DIM ** -0.5)
    vm = jnp.einsum('bshd,hde->bshe', x_m.reshape(B, S, M_HEADS, M_HEAD_DIM).astype(f32), w_vm.astype(f32))
    qkv = jnp.concatenate([qm.reshape(B, S, M_WIDTH), km.reshape(B, S, M_WIDTH), vm.reshape(B, S, M_WIDTH)], axis=-1)
    gates = qkv @ w_if.astype(f32) + b_if.astype(f32)
    log_i = gates[..., :M_HEADS]
    log_f = jax.nn.log_sigmoid(gates[..., M_HEADS:])
    hm = _mlstm_chunkwise(qm, km, vm, log_i, log_f)
    hm = jax.nn.sigmoid(o_m.astype(f32)).reshape(B, S, M_HEADS, M_HEAD_DIM) * hm
    hm = _layer_norm(hm) * mh_norm_w.astype(f32).reshape(M_HEADS, M_HEAD_DIM)
    hm = hm.reshape(B, S, M_WIDTH) + skip_m.astype(f32) * xc
    y_m = (hm * jax.nn.silu(z_m.astype(f32))).astype(dt) @ w_pm

    merged = jax.nn.sigmoid(g_a) * y_att + jax.nn.sigmoid(g_m) * y_m
    out = (merged @ w_out).astype(f32)
    res = alpha * x.astype(f32) + gate[:, None] * out
    return (_layer_norm(res) * ln_g.astype(f32) + ln_b.astype(f32)).astype(dt)


def setup_inputs(seed: int = 0) -> dict:
    key = jax.random.key(seed)
    ks = jax.random.split(key, 24)
    f32 = jnp.float32
    beta = (8.0 * DEPTH) ** -0.25

    def nrm(k, shape, scale):
        return jax.random.normal(k, shape, f32) * scale

    x = jax.random.normal(ks[0], (BATCH, SEQ, D_MODEL), f32)
    c = jax.random.normal(ks[1], (BATCH, D_MODEL), f32)
    offset = jax.random.randint(ks[2], (BATCH, 1), 0, 1024, dtype=jnp.int32)
    positions = (jnp.arange(SEQ, dtype=jnp.int32)[None, :] + offset).astype(jnp.int32)
    w_ada = nrm(ks[3], (DEPTH, D_MODEL, 3 * D_MODEL), D_MODEL ** -0.5)
    b_ada = nrm(ks[4], (DEPTH, 3 * D_MODEL), 0.02)
    w_in = nrm(ks[5], (DEPTH, D_MODEL, IN_WIDTH), D_MODEL ** -0.5)
    conv_w = nrm(ks[6], (DEPTH, M_CONV, M_WIDTH), M_CONV ** -0.5)
    conv_b = nrm(ks[7], (DEPTH, M_WIDTH), 0.02)
    w_qm = nrm(ks[8], (DEPTH, M_HEADS, M_HEAD_DIM, M_HEAD_DIM), M_HEAD_DIM ** -0.5)
    w_km = nrm(ks[9], (DEPTH, M_HEADS, M_HEAD_DIM, M_HEAD_DIM), M_HEAD_DIM ** -0.5)
    w_vm = nrm(ks[10], (DEPTH, M_HEADS, M_HEAD_DIM, M_HEAD_DIM), M_HEAD_DIM ** -0.5)
    w_if = nrm(ks[11], (DEPTH, 3 * M_WIDTH, 2 * M_HEADS), (3 * M_WIDTH) ** -0.5)
    b_i = nrm(ks[12], (DEPTH, M_HEADS), 0.1)
    b_f = 3.0 + nrm(ks[13], (DEPTH, M_HEADS), 0.5)
    b_if = jnp.concatenate([b_i, b_f], axis=-1)
    mh_norm_w = 1.0 + nrm(ks[14], (DEPTH, M_WIDTH), 0.02)
    skip_m = 1.0 + nrm(ks[15], (DEPTH, M_WIDTH), 0.02)
    w_pa = nrm(ks[16], (DEPTH, ATT_KV_WIDTH, D_MODEL), beta * ATT_KV_WIDTH ** -0.5)
    w_pm = nrm(ks[17], (DEPTH, M_WIDTH, D_MODEL), beta * M_WIDTH ** -0.5)
    w_out = nrm(ks[18], (DEPTH, D_MODEL, D_MODEL), beta * D_MODEL ** -0.5)
    ln_g = 1.0 + nrm(ks[19], (DEPTH, D_MODEL), 0.02)
    ln_b = nrm(ks[20], (DEPTH, D_MODEL), 0.02)
    return {'x': x, 'c': c, 'positions': positions, 'w_ada': w_ada, 'b_ada': b_ada,
            'w_in': w_in, 'conv_w': conv_w, 'conv_b': conv_b, 'w_qm': w_qm, 'w_km': w_km,
            'w_vm': w_vm, 'w_if': w_if, 'b_if': b_if, 'mh_norm_w': mh_norm_w, 'skip_m': skip_m,
            'w_pa': w_pa, 'w_pm': w_pm, 'w_out': w_out, 'ln_g': ln_g, 'ln_b': ln_b}


def reference(x, c, positions, w_ada, b_ada, w_in, conv_w, conv_b, w_qm, w_km, w_vm,
              w_if, b_if, mh_norm_w, skip_m, w_pa, w_pm, w_out, ln_g, ln_b):
    for l in range(DEPTH):
        x = _hybrid_layer(x, c, positions, w_ada[l], b_ada[l], w_in[l], conv_w[l], conv_b[l],
                          w_qm[l], w_km[l], w_vm[l], w_if[l], b_if[l], mh_norm_w[l], skip_m[l],
                          w_pa[l], w_pm[l], w_out[l], ln_g[l], ln_b[l])
    return x
```

```python
import numpy as np
from contextlib import ExitStack
import concourse.bass as bass
import concourse.mybir as mybir
from concourse.bass_utils import run_bass_kernel_spmd

F32 = mybir.dt.float32
BF16 = mybir.dt.bfloat16
I32 = mybir.dt.int32
AF = mybir.ActivationFunctionType
ALU = mybir.AluOpType
AX = mybir.AxisListType

ENGS = ("pe", "act", "dve", "pool", "sp")
NDMA = 14
import os
EMBED = bool(int(os.environ.get("K_EMBED", "0")))
_st = os.environ.get("K_STRICT", "none")
STRICT = set(ENGS) if _st == "all" else set(x for x in _st.split(",") if x)
NDMA_SP = 8
P = 128
D = 1024
BLK = 512
NEG = -30000.0
PI_LO = 3.1415925
TWO_PI = 6.2831853
SBUF_BASE = 16512
SBUF_TOP = 229376


class Sched:
    def __init__(self, nc):
        self.nc = nc
        self.ops = {e: [] for e in ENGS}
        self.cnt = {e: 0 for e in ENGS}
        self.known = {e: {} for e in ENGS}
        self.lastw = {}
        self.rd = {}
        self.dma_cnt = [0] * NDMA
        self.dma_rr_sp = 0
        self.dma_rr_pl = 0

    def _deps(self, eng, reads, writes, extra=()):
        deps = {}

        def add(tok):
            if tok is None:
                return
            k, v = tok
            if deps.get(k, 0) < v:
                deps[k] = v

        for t in extra:
            add(t)
        for r in reads:
            add(self.lastw.get(r))
        for w in writes:
            add(self.lastw.get(w))
            for k, v in self.rd.get(w, {}).items():
                add((k, v))
        waits = []
        for k, v in deps.items():
            if k == "pe" and eng == "pe":
                continue
            if self.known[eng].get(k, 0) >= v:
                continue
            self.known[eng][k] = v
            waits.append((k, v))
        return waits

    def _mark(self, tok, reads, writes):
        k, v = tok
        for r in reads:
            d = self.rd.setdefault(r, {})
            if d.get(k, 0) < v:
                d[k] = v
        for w in writes:
            self.lastw[w] = tok
            self.rd[w] = {}

    def op(self, eng, fn, reads=(), writes=(), inc=True):
        assert inc or eng == "pe"
        extra = self._all_tokens(sp_only=True) if eng in STRICT else ()
        waits = self._deps(eng, reads, writes, extra)
        if inc:
            self.cnt[eng] += 1
            tok = (eng, self.cnt[eng])
        else:
            tok = (eng, self.cnt[eng] + 1)
        self._mark(tok, reads, writes)
        self.ops[eng].append((fn, waits, eng if inc else None))

    def dma(self, fn, reads=(), writes=(), queue="sp"):
        if queue == "sp":
            i = self.dma_rr_sp
            self.dma_rr_sp = (i + 1) % NDMA_SP
        else:
            i = NDMA_SP + self.dma_rr_pl
            self.dma_rr_pl = (self.dma_rr_pl + 1) % (NDMA - NDMA_SP)
        key = ("dma", i)
        prev = self.dma_cnt[i]
        extra = [(key, prev)] if prev else []
        if queue in STRICT or (queue == "pool" and "poolq" in STRICT):
            extra = extra + self._all_tokens(sp_only=True)
        waits = self._deps(queue, reads, writes, extra)
        self.dma_cnt[i] = prev + 16
        tok = (key, prev + 16)
        self._mark(tok, reads, writes)
        self.ops[queue].append((fn, waits, key))

    def _all_tokens(self, sp_only=False):
        toks = []
        for i in range(NDMA_SP if sp_only else NDMA):
            if self.dma_cnt[i]:
                toks.append((("dma", i), self.dma_cnt[i]))
        for e in ENGS:
            if e != "sp" and self.cnt[e]:
                toks.append((e, self.cnt[e]))
        return toks

    def snapshot(self):
        return self._all_tokens(sp_only=True)

    def barrier(self):
        self.wait_tokens(self._all_tokens(sp_only=True))

    def wait_tokens(self, toks):
        for e in ("pe", "act", "dve", "pool", "sp"):
            waits = []
            for k, v in toks:
                if k == e or (e == "sp" and isinstance(k, tuple)):
                    continue
                if self.known[e].get(k, 0) >= v:
                    continue
                self.known[e][k] = v
                waits.append((k, v))
            self.ops[e].append((None, waits, None))

    def finish(self):
        self.ops["sp"].append((None, self._all_tokens(), None))

    def emit(self):
        nc = self.nc
        with ExitStack() as st:
            sems = {}
            for e in ENGS:
                sems[e] = st.enter_context(nc.semaphore("s_" + e))
            for i in range(NDMA):
                sems[("dma", i)] = st.enter_context(nc.semaphore("s_dma%d" % i))
            block = st.enter_context(nc.Block())

            def run(eng, e):
                for fn, waits, inc in self.ops[e]:
                    emb = None
                    if EMBED and fn is not None and waits and e == "pe":
                        emb = waits[-1]
                        waits = waits[:-1]
                    for k, v in waits:
                        eng.wait_ge(sems[k], v)
                    if fn is None:
                        continue
                    ins = fn(eng)
                    if emb is not None:
                        ins._wait_ge(sems[emb[0]], emb[1])
                    if inc is not None:
                        ins.then_inc(sems[inc], 16 if isinstance(inc, tuple) else 1)

            @block.tensor
            def _(eng):
                run(eng, "pe")

            @block.scalar
            def _(eng):
                run(eng, "act")

            @block.vector
            def _(eng):
                run(eng, "dve")

            @block.gpsimd
            def _(eng):
                run(eng, "pool")

            @block.sync
            def _(eng):
                run(eng, "sp")


def _dsize(dt):
    return 4 if dt in (F32, I32) else 2


class Arena:
    def __init__(self, nc):
        self.nc = nc
        self.off = SBUF_BASE
        self.n = 0
        self.peak = SBUF_BASE

    def alloc(self, name, shape, dt):
        nbytes = int(np.prod(shape[1:])) * _dsize(dt)
        nbytes = (nbytes + 31) // 32 * 32
        assert self.off + nbytes <= SBUF_TOP, (name, self.off, nbytes)
        self.n += 1
        t = self.nc.alloc_sbuf_tensor_at("%s_%d" % (name, self.n), list(shape), dt, offset=self.off)
        self.off += nbytes
        self.peak = max(self.peak, self.off)
        return t

    def mark(self):
        return self.off

    def release(self, m):
        self.off = m


CK_Q0, CK_Q1, CK_Q2, CK_K, CK_V, CK_ZA, CK_XM0, CK_XM1, CK_ZM0, CK_ZM1, CK_OM0, CK_OM1, \
    CK_GA0, CK_GA1, CK_GM0, CK_GM1, CK_PA, CK_PM0, CK_PM1, CK_OUT0, CK_OUT1 = range(21)
NCHUNK = 21
STREAM_ORDER = [CK_K, CK_V, CK_Q0, CK_Q1, CK_Q2, CK_ZA, CK_XM0, CK_XM1, CK_ZM0, CK_ZM1,
                CK_OM0, CK_OM1, CK_GA0, CK_GA1, CK_GM0, CK_GM1, CK_PA, CK_PM0, CK_PM1,
                CK_OUT0, CK_OUT1]


def build_nc(S_LEN=4096, dbg=False):
    NBLK = S_LEN // BLK
    NTILE = S_LEN // P
    nc = bass.Bass("TRN2", target_bir_lowering=False)

    def din(name, shape, dt=F32):
        return nc.dram_tensor(name, list(shape), dt, kind="ExternalInput").ap()

    x_d = din("x", [S_LEN, D])
    cT_d = din("cT", [P, 8])
    pos_d = din("posT", [P, NTILE], I32)
    w_ada_d = din("w_ada", [D, 3 * D])
    b_ada_col_d = din("b_ada_col", [P, 24])
    b_ada_d = din("b_ada", [1, 3 * D])
    w_in_d = din("w_in", [D, 8192])
    convw_d = din("convw_col", [P, 8, 4])
    convb_d = din("convb_col", [P, 8])
    w_qm_d = din("w_qm", [4, 256, 256])
    w_km_d = din("w_km", [4, 256, 256])
    w_vm_d = din("w_vm", [4, 256, 256])
    w_qmT_d = din("w_qmT", [4, 256, 256])
    w_kmT_d = din("w_kmT", [4, 256, 256])
    w_vmT_d = din("w_vmT", [4, 256, 256])
    w_if_d = din("w_if", [3072, 8])
    b_if_d = din("b_if", [1, 8])
    mhw_d = din("mhw_col", [P, 8])
    skip_d = din("skip_col", [P, 8])
    w_pa_d = din("w_pa", [512, D])
    w_pm_d = din("w_pm", [D, D])
    w_out_d = din("w_out", [D, D])
    ln_g_d = din("ln_g", [1, D])
    ln_b_d = din("ln_b", [1, D])
    invf_d = din("invf", [P, 64])
    mg0_d = din("mg0", [P, 4, 640])
    mg12_d = din("mg12", [P, 896])
    cmat_d = din("cmat", [P, 4, 128])
    y_d = nc.dram_tensor("y", [S_LEN, D], F32, kind="ExternalOutput").ap()
    wbf_d = nc.dram_tensor("wbf", [NCHUNK, P, 4096], BF16).ap()
    dumps = {}

    def dump_decl(name, shape, dt=F32):
        dumps[name] = nc.dram_tensor("dbg_" + name, list(shape), dt, kind="ExternalOutput").ap()
        return dumps[name]

    S = Sched(nc)
    A = Arena(nc)
    with ExitStack() as st:
        ps_s = st.enter_context(nc.psum_tensor("ps_s", [P, 1536], F32))
        pb3 = st.enter_context(nc.psum_tensor("pb3", [P, 512], F32))
        pb4 = st.enter_context(nc.psum_tensor("pb4", [P, 512], F32))
        pb5 = st.enter_context(nc.psum_tensor("pb5", [P, 512], F32))
        ptA = st.enter_context(nc.psum_tensor("ptA", [P, 1024], BF16))
        ptB = st.enter_context(nc.psum_tensor("ptB", [P, 1024], BF16))
        PB = [ps_s[:, 0:512], ps_s[:, 512:1024], ps_s[:, 1024:1536], pb3[:], pb4[:], pb5[:]]
        PBN = ["PB0", "PB1", "PB2", "PB3", "PB4", "PB5"]
        PS_S = ["PB0", "PB1", "PB2"]

        cmat = A.alloc("cmat", [P, 4, 128], F32)
        ident_f = cmat[:, 0, :]
        ones_f = cmat[:, 1, :]
        tri_f = cmat[:, 2, :]
        ident_b = A.alloc("ident_b", [P, 128], BF16)
        triU_b = A.alloc("triU_b", [P, 128], BF16)
        mg0 = A.alloc("mg0", [P, 4, 640], BF16)
        mg12 = A.alloc("mg12", [P, 896], BF16)
        invf = A.alloc("invf", [P, 64], F32)
        pos_i = A.alloc("pos_i", [P, NTILE], I32)
        pos_f = A.alloc("pos_f", [P, NTILE], F32)
        cols = A.alloc("cols", [P, 64], F32)
        pi_col = cols[:, 0:1]
        eps_col = cols[:, 1:2]
        one_col = cols[:, 2:3]
        cT = A.alloc("cT", [P, 8], F32)
        siluc = A.alloc("siluc", [P, 8], F32)
        sc1 = A.alloc("sc1", [P, 8], F32)
        sh = A.alloc("sh", [P, 8], F32)
        badac = A.alloc("badac", [P, 24], F32)
        gate_bc = A.alloc("gate_bc", [P, D], F32)
        lng_bc = A.alloc("lng_bc", [P, D], F32)
        lnb_bc = A.alloc("lnb_bc", [P, D], F32)
        mhw = A.alloc("mhw", [P, 8], F32)
        skipc = A.alloc("skipc", [P, 8], F32)
        convw = A.alloc("convw", [P, 8, 4], F32)
        convb = A.alloc("convb", [P, 8], F32)
        bif_bc = A.alloc("bif_bc", [P, 8], F32)
        AB = A.alloc("AB", [P, 16, 8], BF16)
        wq_b = A.alloc("wq_b", [P, 4, 2, 256], BF16)
        wk_b = A.alloc("wk_b", [P, 4, 2, 256], BF16)
        wv_b = A.alloc("wv_b", [P, 4, 2, 256], BF16)
        KT = A.alloc("KT", [P, 4, 5 * BLK], BF16)
        V1 = A.alloc("V1", [P, 20, 512], BF16)
        V0 = A.alloc("V0", [P, 8, 512], BF16)
        Cst = A.alloc("Cst", [P, 8, 257], F32)
        Cb = A.alloc("Cb", [P, 8, 257], BF16)
        mp4 = A.alloc("mp4", [P, 20], F32)
        halo = A.alloc("halo", [P, 8, 4], BF16)
        wst = [A.alloc("wst0", [P, 8, 512], BF16), A.alloc("wst1", [P, 8, 512], BF16)]
        hT = A.alloc("hT", [P, 8, BLK], BF16)
        ogT = A.alloc("ogT", [P, 4, BLK], BF16)
        yminT = A.alloc("yminT", [P, 8, BLK], BF16)
        phase_mark = A.mark()

        def mm(out, lhsT, rhs, start, stop, reads, writes, inc=None):
            if inc is None:
                inc = stop
            S.op("pe", lambda e: e.matmul(out, lhsT, rhs, start=start, stop=stop),
                 reads=reads, writes=writes, inc=inc)

        def tr(out, in_, ident, reads, writes, inc=True):
            S.op("pe", lambda e: e.transpose(out, in_, ident), reads=reads, writes=writes, inc=inc)

        def act(out, in_, func, reads, writes, bias=None, scale=1.0, accum=None):
            kw = {}
            if bias is not None:
                kw["bias"] = bias
            if accum is not None:
                kw["accum_out"] = accum
            S.op("act", lambda e: e.activation(out, in_, func, scale=scale, **kw),
                 reads=reads, writes=writes)

        def cp(eng, out, in_, reads, writes):
            if eng == "act":
                S.op("act", lambda e: e.copy(out, in_), reads=reads, writes=writes)
            else:
                S.op(eng, lambda e: e.tensor_copy(out, in_), reads=reads, writes=writes)

        def tt(eng, out, a, b, op, reads, writes):
            S.op(eng, lambda e: e.tensor_tensor(out, a, b, op), reads=reads, writes=writes)

        def ts(eng, out, a, s1, s2, op0, op1, reads, writes):
            if s2 is None:
                S.op(eng, lambda e: e.tensor_scalar(out, a, s1, None, op0), reads=reads, writes=writes)
            else:
                S.op(eng, lambda e: e.tensor_scalar(out, a, s1, s2, op0, op1), reads=reads, writes=writes)

        def stt(out, a, s, b, op0, op1, reads, writes):
            S.op("dve", lambda e: e.scalar_tensor_tensor(out, a, s, b, op0, op1), reads=reads, writes=writes)

        def dma(out, in_, reads, writes, queue="sp"):
            S.dma(lambda e: e.dma_start(out=out, in_=in_), reads=reads, writes=writes, queue=queue)

        def dump(name, sb_ap, reads, shape, dt=F32):
            if not dbg:
                return
            d = dump_decl(name, shape, dt)
            dma(d, sb_ap, reads, [])

        def cast_chunk(ck):
            if ck <= CK_GM1:
                src = w_in_d[:, ck * 512:(ck + 1) * 512].rearrange("(k p) n -> p k n", p=P)
                dst = wbf_d[ck].rearrange("p (k n) -> p k n", k=8)
            elif ck == CK_PA:
                src = w_pa_d.rearrange("(k p) n -> p k n", p=P)
                dst = wbf_d[ck].rearrange("p (k n) -> p k n", k=4)
            elif ck in (CK_PM0, CK_PM1):
                c = ck - CK_PM0
                src = w_pm_d[:, c * 512:(c + 1) * 512].rearrange("(k p) n -> p k n", p=P)
                dst = wbf_d[ck].rearrange("p (k n) -> p k n", k=8)
            else:
                c = ck - CK_OUT0
                src = w_out_d[:, c * 512:(c + 1) * 512].rearrange("(k p) n -> p k n", p=P)
                dst = wbf_d[ck].rearrange("p (k n) -> p k n", k=8)
            dma(dst, src, [], [("wbf", ck)], queue="pool")

        dma(cmat[:], cmat_d, [], ["cmat"])
        dma(invf[:], invf_d, [], ["invf"])
        dma(pos_i[:], pos_d, [], ["pos_i"])
        dma(cT[:], cT_d, [], ["cT"])
        dma(badac[:], b_ada_col_d, [], ["badac"])
        dma(mhw[:], mhw_d, [], ["mhw"])
        dma(skipc[:], skip_d, [], ["skipc"])
        dma(convw[:], convw_d, [], ["convw"])
        dma(convb[:], convb_d, [], ["convb"])
        dma(bif_bc[:], b_if_d[0:1, :].partition_broadcast(P), [], ["bif_bc"])
        dma(lng_bc[:], ln_g_d[0:1, :].partition_broadcast(P), [], ["lng_bc"])
        dma(lnb_bc[:], ln_b_d[0:1, :].partition_broadcast(P), [], ["lnb_bc"])
        dma(gate_bc[:], b_ada_d[0:1, 2 * D:3 * D].partition_broadcast(P), [], ["gate_bc"])
        dma(mg0[:], mg0_d, [], ["mg0"], queue="pool")
        dma(mg12[:], mg12_d, [], ["mg12"], queue="pool")
        dma(wq_b[:], w_qm_d.rearrange("h (c p) e -> p h c e", p=P), [], ["wq_b"], queue="pool")
        dma(wk_b[:], w_km_d.rearrange("h (c p) e -> p h c e", p=P), [], ["wk_b"], queue="pool")
        dma(wv_b[:], w_vm_d.rearrange("h (c p) e -> p h c e", p=P), [], ["wv_b"], queue="pool")
        for ck in STREAM_ORDER:
            cast_chunk(ck)

        S.op("pool", lambda e: e.memset(cols[:, 0:1], PI_LO), writes=["cols"])
        S.op("pool", lambda e: e.memset(cols[:, 1:2], 1e-5), writes=["cols"])
        S.op("pool", lambda e: e.memset(cols[:, 2:3], 1.0), writes=["cols"])
        cp("dve", ident_b[:], ident_f, ["cmat"], ["ident_b"])
        cp("dve", triU_b[:], tri_f, ["cmat"], ["triU_b"])
        cp("dve", pos_f[:], pos_i[:], ["pos_i"], ["pos_f"])
        S.op("pool", lambda e: e.memset(Cst[:], 0.0), writes=["Cst"])
        S.op("pool", lambda e: e.memset(mp4[:], 0.0), writes=["mp4"])
        S.op("pool", lambda e: e.memset(halo[:], 0.0), writes=["halo"])

        pm = A.mark()
        stg = [A.alloc("stg0", [P, 8, 512], F32), A.alloc("stg1", [P, 8, 512], F32)]
        sbc = A.alloc("sbc", [P, 8, 128], F32)
        act(siluc[:], cT[:], AF.Silu, ["cT"], ["siluc"])
        for k in range(8):
            ts("dve", sbc[:, k, :], ones_f, siluc[:, k:k + 1], None, ALU.mult, None,
               ["cmat", "siluc"], [("sbc", k)])
        for piece in range(6):
            sgi = piece % 2
            dma(stg[sgi][:], w_ada_d[:, piece * 512:(piece + 1) * 512].rearrange("(k p) n -> p k n", p=P),
                [], [("stg", sgi)])
            if piece < 4:
                for jj in range(4):
                    j = piece * 4 + jj
                    for k in range(8):
                        mm(PB[3][:, j:j + 1], stg[sgi][:, k, jj * 128:(jj + 1) * 128], siluc[:, k:k + 1],
                           k == 0, k == 7, [("stg", sgi), "siluc"], ["PB3"])
            else:
                g = piece - 4
                for k in range(8):
                    mm(PB[4 + g], sbc[:, k, :], stg[sgi][:, k, :], k == 0, k == 7,
                       [("stg", sgi)] + [("sbc", kk) for kk in range(8)], [PBN[4 + g]])
                tt("dve", gate_bc[:, g * 512:(g + 1) * 512], PB[4 + g], gate_bc[:, g * 512:(g + 1) * 512], ALU.add,
                   [PBN[4 + g], "gate_bc"], ["gate_bc"])
        tt("dve", sh[:], PB[3][:, 0:8], badac[:, 0:8], ALU.add, ["PB3", "badac"], ["sh"])
        tt("dve", sc1[:], PB[3][:, 8:16], badac[:, 8:16], ALU.add, ["PB3", "badac"], ["sc1"])
        ts("dve", sc1[:], sc1[:], 1.0, None, ALU.add, None, ["sc1"], ["sc1"])

        A.release(pm)
        wif_s = A.alloc("wif_s", [P, 24, 8], F32)
        wT_s = A.alloc("wT_s", [P, 3, 4, 2, 256], F32)
        alias_w = [("stg", 0), ("stg", 1)] + [("sbc", k) for k in range(8)]
        dma(wif_s[:], w_if_d.rearrange("(c p) g -> p c g", p=P), [], ["wif_s"] + alias_w)
        for mi, wd in enumerate((w_qmT_d, w_kmT_d, w_vmT_d)):
            dma(wT_s[:, mi], wd.rearrange("h (c p) d -> p h c d", p=P), [], [("wT_s", mi)] + alias_w)
        ts("dve", wif_s[:, 8:16, :], wif_s[:, 8:16, :], 1.0 / 16.0, None, ALU.mult, None, ["wif_s"], ["wif_s"])
        for h in range(4):
            for dc in range(2):
                j = 2 * h + dc
                terms = [(0, 0 + 2 * h + ec, ec) for ec in range(2)] + [(1, 8 + 2 * h + ec, ec) for ec in range(2)]
                for ti, (mi, wc, ec) in enumerate(terms):
                    mm(PB[3][:, 32 + j * 8:32 + (j + 1) * 8], wT_s[:, mi, h, ec, dc * 128:(dc + 1) * 128], wif_s[:, wc, :],
                       ti == 0, ti == 3, [("wT_s", mi), "wif_s"], ["PB3"])
                for ec in range(2):
                    mm(PB[3][:, 96 + j * 8:96 + (j + 1) * 8], wT_s[:, 2, h, ec, dc * 128:(dc + 1) * 128],
                       wif_s[:, 16 + 2 * h + ec, :], ec == 0, ec == 1, [("wT_s", 2), "wif_s"], ["PB3"])
        cp("dve", AB[:, 0:8, :], PB[3][:, 32:96].rearrange("p (j g) -> p j g", g=8), ["PB3"], ["AB"])
        cp("dve", AB[:, 8:16, :], PB[3][:, 96:160].rearrange("p (j g) -> p j g", g=8), ["PB3"], ["AB"])
        S.barrier()
        A.release(phase_mark)

        seq = [(a, ck) for a in range(NBLK) for ck in STREAM_ORDER]
        wstate = {"next_load": 0, "cur": 0}

        def w_issue():
            i = wstate["next_load"]
            if i >= len(seq):
                return
            ck = seq[i][1]
            slot = i % 2
            dma(wst[slot][:].rearrange("p k n -> p (k n)"), wbf_d[ck], [("wbf", ck)], [("wst", slot)])
            wstate["next_load"] = i + 1

        def w_cur(a, ck):
            i = wstate["cur"]
            assert seq[i] == (a, ck), (seq[i], a, ck)
            return wst[i % 2], ("wst", i % 2)

        def w_done():
            wstate["cur"] += 1
            w_issue()

        w_issue()
        w_issue()

        hT_all = [("hT", t) for t in range(4)]
        rstate = {"i": 0}

        def rot():
            i = rstate["i"]
            rstate["i"] = (i + 1) % 6
            return PB[i], PBN[i]

        def proj_tok(ps, psn, w, wn, col_ap_fn, hreads, ncols=512, c0=0):
            for k in range(8):
                mm(ps, col_ap_fn(k), w[:, k, c0:c0 + ncols], k == 0, k == 7, hreads + [wn], [psn])

        def proj_feat(ps, psn, w, wn, c0):
            for k in range(8):
                mm(ps, w[:, k, c0:c0 + 128], hT[:, k, :], k == 0, k == 7, hT_all + [wn], [psn])

        def emit_ln(ab, xt, bn, mv, rstd):
            t0 = ab * BLK
            for t in range(4):
                xs = xt[t % 2]
                xn = ("xt", t % 2)
                dma(xs[:], x_d[t0 + t * P:t0 + (t + 1) * P, :], [], [xn])
                bi = t % 2
                for hf in range(2):
                    S.op("dve", lambda e, o=bn[:, bi, hf, :], i=xs[:, hf * 512:(hf + 1) * 512]: e.bn_stats(o, i),
                         reads=[xn], writes=[("bn", bi)])
                S.op("dve", lambda e, o=mv[:, bi, :], i=bn[:, bi].rearrange("p a b -> p (a b)"): e.bn_aggr(o, i),
                     reads=[("bn", bi)], writes=[("mv", bi)])
                act(rstd[:, bi:bi + 1], mv[:, bi, 1:2], AF.Sqrt, [("mv", bi), "cols"], [("rstd", bi)], bias=eps_col)
                S.op("dve", lambda e, o=rstd[:, bi:bi + 1]: e.reciprocal(o, o), reads=[("rstd", bi)], writes=[("rstd", bi)])
                ts("dve", xs[:], xs[:], mv[:, bi, 0:1], rstd[:, bi:bi + 1], ALU.subtract, ALU.mult,
                   [xn, ("mv", bi), ("rstd", bi)], [xn])
                for half in range(2):
                    pb, pbn = rot()
                    for kk in range(4):
                        k = half * 4 + kk
                        tr(pb[:, kk * 128:(kk + 1) * 128], xs[:, k * 128:(k + 1) * 128], ident_f,
                           [xn, "cmat"], [pbn], inc=(kk == 3))
                    for kk in range(4):
                        k = half * 4 + kk
                        act(hT[:, k, t * P:(t + 1) * P], pb[:, kk * 128:(kk + 1) * 128], AF.Identity,
                            [pbn, "sc1", "sh"], [("hT", t)], bias=sh[:, k:k + 1], scale=sc1[:, k:k + 1])

        for a in range(NBLK):
            t0 = a * BLK
            A.release(phase_mark)
            if a > 0:
                S.wait_tokens(fence1)
            t1 = [A.alloc("t1_0", [P, 512], F32), A.alloc("t1_1", [P, 512], F32)]
            t2 = [A.alloc("t2_0", [P, 512], F32), A.alloc("t2_1", [P, 512], F32)]
            qr = [A.alloc("qr0", [P, 512], BF16), A.alloc("qr1", [P, 512], BF16)]
            QT = A.alloc("QT", [P, 12, BLK], BF16)
            zs = A.alloc("zs", [P, 4, 512], BF16)
            cosT = A.alloc("cosT", [P, 4, 64], F32)
            sinT = A.alloc("sinT", [P, 4, 64], F32)
            angT = A.alloc("angT", [P, 4, 64], F32)
            angT2 = A.alloc("angT2", [P, 4, 64], F32)
            angQ = A.alloc("angQ", [P, 4, 64], F32)
            angI = A.alloc("angI", [P, 4, 64], I32)
            b_end = A.off
            sm = [A.alloc("sm%d" % i, [P, 1536], F32) for i in range(3)]
            Pb = [A.alloc("Pb%d" % i, [P, 1536], BF16) for i in range(3)]
            PT = [A.alloc("PT0", [P, 12, 128], BF16), A.alloc("PT1", [P, 12, 128], BF16)]
            stat = A.alloc("stat", [P, 32], F32)
            og = A.alloc("og", [P, 512], BF16)
            if a == 0:
                xt = [A.alloc("xt0", [P, D], F32), A.alloc("xt1", [P, D], F32)]
                bn = A.alloc("bn", [P, 2, 2, 6], F32)
                mv = A.alloc("mv", [P, 2, 2], F32)
                rstd = A.alloc("rstd", [P, 2], F32)

            tt("dve", angT[:], pos_f[:, 4 * a:4 * a + 4].unsqueeze(2).to_broadcast([P, 4, 64]),
               invf[:].unsqueeze(1).to_broadcast([P, 4, 64]), ALU.mult, ["pos_f", "invf"], ["angT"])
            INV2PI = 1.0 / 6.283185307179586
            for (dst, off, nm) in ((sinT, 0.0, "sinT"), (cosT, 0.5 * 3.14159265358979, "cosT")):
                if off != 0.0:
                    ts("dve", angT2[:], angT[:], off, None, ALU.add, None, ["angT"], ["angT2"])
                    src, srcn = angT2, "angT2"
                else:
                    src, srcn = angT, "angT"
                ts("dve", angQ[:], src[:], INV2PI, None, ALU.mult, None, [srcn], ["angQ"])
                cp("dve", angI[:], angQ[:], ["angQ"], ["angI"])
                cp("dve", angQ[:], angI[:], ["angI"], ["angQ"])
                stt(angQ[:], angQ[:], -6.283185307179586, src[:], ALU.mult, ALU.add, ["angQ", srcn], ["angQ"])
                ts("dve", angQ[:], angQ[:], PI_LO, -PI_LO, ALU.min, ALU.max, ["angQ"], ["angQ"])
                act(dst[:], angQ[:], AF.Sin, ["angQ"], [nm])

            if a == 0:
                emit_ln(0, xt, bn, mv, rstd)
            if a == 0:
                dump("hT", hT[:], hT_all, [P, 8, BLK], BF16)

            kslot = (a % 5) * BLK

            def rope_tile(ps, psn, t, scale, out_bf, outn, bi):
                psv = ps.rearrange("p (h f j) -> p h f j", h=4, f=2)
                t1v = t1[bi][:].rearrange("p (h f j) -> p h f j", h=4, f=2)
                t2v = t2[bi][:].rearrange("p (h f j) -> p h f j", h=4, f=2)
                ov = out_bf.rearrange("p (h f j) -> p h f j", h=4, f=2)
                cb = cosT[:, t, :].unsqueeze(1).to_broadcast([P, 8, 64])
                sb1 = sinT[:, t, :].unsqueeze(1).to_broadcast([P, 4, 64])
                stt(t1[bi][:].rearrange("p (g j) -> p g j", j=64), ps.rearrange("p (g j) -> p g j", j=64), scale, cb,
                    ALU.mult, ALU.mult, [psn, "cosT"], [("t1", bi)])
                stt(t2v[:, :, 0, :], psv[:, :, 1, :], scale, sb1, ALU.mult, ALU.mult, [psn, "sinT"], [("t2a", bi)])
                stt(t2v[:, :, 1, :], psv[:, :, 0, :], scale, sb1, ALU.mult, ALU.mult, [psn, "sinT"], [("t2b", bi)])
                tt("dve", ov[:, :, 0, :], t1v[:, :, 0, :], t2v[:, :, 0, :], ALU.subtract,
                   [("t1", bi), ("t2a", bi)], [(outn, "a")])
                tt("dve", ov[:, :, 1, :], t1v[:, :, 1, :], t2v[:, :, 1, :], ALU.add,
                   [("t1", bi), ("t2b", bi)], [(outn, "b")])

            w, wn = w_cur(a, CK_K)
            for t in range(4):
                bi = t % 2
                ps, psn = rot()
                proj_tok(ps, psn, w, wn, lambda k, t=t: hT[:, k, t * P:(t + 1) * P], [("hT", t)])
                rope_tile(ps, psn, t, 1.0, qr[bi][:], ("qr", bi), bi)
                for h in range(4):
                    tr(ptA[:, h * 128:(h + 1) * 128], qr[bi][:, h * 128:(h + 1) * 128], ident_b[:],
                       [(("qr", bi), "a"), (("qr", bi), "b"), "ident_b"], ["ptA"], inc=(h == 3))
                cp("act", KT[:, :, kslot + t * P:kslot + (t + 1) * P], ptA[:, 0:512].rearrange("p (h j) -> p h j", h=4),
                   ["ptA"], [("KT", a % 5)])
            w_done()
            w, wn = w_cur(a, CK_V)
            for t in range(4):
                bi = t % 2
                ps, psn = rot()
                proj_tok(ps, psn, w, wn, lambda k, t=t: hT[:, k, t * P:(t + 1) * P], [("hT", t)])
                cp("act", V0[:, (4 * a + t) % 8, :], ps, [psn], [("V0", (4 * a + t) % 8)])
            for r in range(4):
                bi = r % 2
                ps, psn = rot()
                proj_tok(ps, psn, w, wn, lambda k, r=r: hT[:, k, r:BLK:4], hT_all)
                cp("act", V1[:, (a % 5) * 4 + r, :], ps, [psn], [("V1", (a % 5) * 4 + r)])
            w_done()
            for g in range(3):
                w, wn = w_cur(a, CK_Q0 + g)
                for t in range(4):
                    bi = t % 2
                    ps, psn = rot()
                    proj_tok(ps, psn, w, wn, lambda k, t=t: hT[:, k, t * P:(t + 1) * P], [("hT", t)])
                    rope_tile(ps, psn, t, 128.0 ** -0.5, qr[bi][:], ("qr", bi), bi)
                    for h in range(4):
                        tr(ptA[:, h * 128:(h + 1) * 128], qr[bi][:, h * 128:(h + 1) * 128], ident_b[:],
                           [(("qr", bi), "a"), (("qr", bi), "b"), "ident_b"], ["ptA"], inc=(h == 3))
                    cp("act", QT[:, 4 * g:4 * g + 4, t * P:(t + 1) * P], ptA[:, 0:512].rearrange("p (h j) -> p h j", h=4),
                       ["ptA"], [("QT", g)])
                w_done()
            w, wn = w_cur(a, CK_ZA)
            for r in range(4):
                bi = r % 2
                ps, psn = rot()
                proj_tok(ps, psn, w, wn, lambda k, r=r: hT[:, k, r:BLK:4], hT_all)
                act(zs[:, r, :], ps, AF.Silu, [psn], [("zs", r)])
            w_done()
            if a == 0:
                dump("QT", QT[:], [("QT", g) for g in range(3)], [P, 12, BLK], BF16)
                dump("KT", KT[:, :, 0:BLK], [("KT", 0)], [P, 4, BLK], BF16)
                dump("V1", V1[:, 0:4, :], [("V1", r) for r in range(4)], [P, 4, 512], BF16)

            def unit_tiles(r, h):
                tiles = []
                for j in range(0 if a > 0 else 1, 5):
                    tile_idx = 4 * a + j - 1
                    ring = tile_idx // 4 % 5
                    kc = ring * BLK + (tile_idx % 4) * P
                    tiles.append((0, KT[:, h, kc:kc + P], V0[:, tile_idx % 8, h * 128:(h + 1) * 128],
                                  ("V0", tile_idx % 8), ("KT", ring)))
                n0 = len(tiles)
                for aa in range(max(0, a - 1), a + 1):
                    ring = aa % 5
                    tiles.append((1, KT[:, h, ring * BLK + r:(ring + 1) * BLK:4], V1[:, ring * 4 + r, h * 128:(h + 1) * 128],
                                  ("V1", ring * 4 + r), ("KT", ring)))
                n1 = len(tiles) - n0
                for aa in range(max(0, a - 4), a + 1):
                    ring = aa % 5
                    tiles.append((2, KT[:, h, ring * BLK + r:(ring + 1) * BLK:4], V1[:, ring * 4 + r, h * 128:(h + 1) * 128],
                                  ("V1", ring * 4 + r), ("KT", ring)))
                n2 = len(tiles) - n0 - n1
                return tiles, n0, n1, n2

            def a_qk(r, h):
                tiles, n0, n1, n2 = unit_tiles(r, h)
                for ti, (g, kap, vap, vn, kn) in enumerate(tiles):
                    mm(ps_s[:, ti * 128:(ti + 1) * 128], QT[:, 4 * g + h, r:BLK:4], kap, True, True,
                       [("QT", g), kn], [PS_S[ti // 4]])

            def a_softmax(r, h, ub):
                tiles, n0, n1, n2 = unit_tiles(r, h)
                ncol = len(tiles) * 128
                c0 = 0
                segs = [(n0, mg0[:, r, 640 - n0 * 128:640], "mg0"),
                        (n1, mg12[:, 256 - n1 * 128:256], "mg12"),
                        (n2, mg12[:, 896 - n2 * 128:896] if a < 4 else mg12[:, 256:896], "mg12")]
                for (nseg, mk, mkn) in segs:
                    w_ = nseg * 128
                    banks = sorted(set(PS_S[c // 512] for c in range(c0, c0 + w_, 128)))
                    tt("dve", sm[ub][:, c0:c0 + w_], ps_s[:, c0:c0 + w_], mk, ALU.add, banks + [mkn], [("sm", ub, c0 // 128)])
                    c0 += w_
                smr = [("sm", ub, 0), ("sm", ub, n0), ("sm", ub, n0 + n1)]
                S.op("dve", lambda e, o=stat[:, 16 + ub:17 + ub], i=sm[ub][:, 0:ncol]: e.reduce_max(o, i, axis=AX.X),
                     reads=smr, writes=[("stat0", ub)])
                ts("dve", stat[:, 20 + ub:21 + ub], stat[:, 16 + ub:17 + ub], -1.0, None, ALU.mult, None, [("stat0", ub)], [("stat1", ub)])
                act(Pb[ub][:, 0:ncol], sm[ub][:, 0:ncol], AF.Exp, smr + [("stat1", ub)], [("Pb", ub), ("den", r % 2, h)],
                    bias=stat[:, 20 + ub:21 + ub], accum=stat[:, (r % 2) * 4 + h:(r % 2) * 4 + h + 1])

            def a_trans(r, h, ub, pb_):
                tiles, n0, n1, n2 = unit_tiles(r, h)
                nt = len(tiles)
                for ti in range(nt):
                    pt = ptA if ti < 8 else ptB
                    ptn = "ptA" if ti < 8 else "ptB"
                    o = (ti % 8) * 128
                    last = (ti == nt - 1) or (ti == 7)
                    tr(pt[:, o:o + 128], Pb[pb_][:, ti * 128:(ti + 1) * 128], ident_b[:], [("Pb", pb_), "ident_b"], [ptn], inc=last)
                nA = min(nt, 8)
                cp("act", PT[ub][:, 0:nA, :], ptA[:, 0:nA * 128].rearrange("p (t j) -> p t j", j=128), ["ptA"], [("PTa", ub)])
                if nt > 8:
                    cp("act", PT[ub][:, 8:nt, :], ptB[:, 0:(nt - 8) * 128].rearrange("p (t j) -> p t j", j=128), ["ptB"], [("PTb", ub)])

            def a_pv(r, h, ub):
                tiles, n0, n1, n2 = unit_tiles(r, h)
                nt = len(tiles)
                po, pon = PB[3 + r % 2], PBN[3 + r % 2]
                for ti, (g, kap, vap, vn, kn) in enumerate(tiles):
                    mm(po[:, h * 128:(h + 1) * 128], PT[ub][:, ti, :], vap, ti == 0, ti == nt - 1,
                       [("PTa", ub), ("PTb", ub), vn], [pon])

            def a_final(r):
                po, pon = PB[3 + r % 2], PBN[3 + r % 2]
                rb = (r % 2) * 4
                S.op("dve", lambda e, o=stat[:, 8 + rb:12 + rb], i=stat[:, rb:rb + 4]: e.reciprocal(o, i),
                     reads=[("den", r % 2, h) for h in range(4)], writes=[("rden", r % 2)])
                for h in range(4):
                    stt(og[:, h * 128:(h + 1) * 128], po[:, h * 128:(h + 1) * 128], stat[:, 8 + rb + h:9 + rb + h],
                        zs[:, r, h * 128:(h + 1) * 128], ALU.mult, ALU.mult, [pon, ("rden", r % 2), ("zs", r)], [("og", h)])
                for h in range(4):
                    tr(PB[5].bitcast(BF16)[:, h * 128:(h + 1) * 128], og[:, h * 128:(h + 1) * 128], ident_b[:], [("og", h), "ident_b"], ["PB5"],
                       inc=(h == 3))
                cp("act", ogT[:, :, r:BLK:4], PB[5].bitcast(BF16)[:, 0:512].rearrange("p (h j) -> p h j", h=4), ["PB5"], [("ogT", r)])

            if a > 0:
                S.wait_tokens(fence2)
            units = [(r, h) for r in range(4) for h in range(4)]
            S.op("pool", lambda e, o=stat[:, 0:8]: e.memset(o, 0.0), writes=[("den", rb, h) for rb in range(2) for h in range(4)])
            def a_sm(ui):
                r, h = units[ui]
                if h == 0 and r >= 2:
                    S.op("pool", lambda e, o=stat[:, (r % 2) * 4:(r % 2) * 4 + 4]: e.memset(o, 0.0),
                         writes=[("den", r % 2, hh) for hh in range(4)])
                a_softmax(r, h, ui % 3)

            a_qk(*units[0])
            a_sm(0)
            a_qk(*units[1])
            a_sm(1)
            for ui, (r, h) in enumerate(units):
                a_trans(r, h, ui % 2, ui % 3)
                if ui + 2 < len(units):
                    a_qk(*units[ui + 2])
                    a_sm(ui + 2)
                a_pv(r, h, ui % 2)
                if h == 3:
                    a_final(r)
            if a == 0:
                dump("ogT", ogT[:], [("ogT", r) for r in range(4)], [P, 4, BLK], BF16)
            if a == 0:
                print('phase ABC end', A.off)
            S.barrier()

            A.release(phase_mark)
            xmT = A.alloc("xmT", [P, 8, 516], BF16)
            xcT = A.alloc("xcT", [P, 8, BLK], BF16)
            zmT = A.alloc("zmT", [P, 8, BLK], BF16)
            om = A.alloc("om", [P, 4, D], BF16)
            qmT = A.alloc("qmT", [P, 8, BLK], BF16)
            kmT = A.alloc("kmT", [P, 8, BLK], BF16)
            cacc = [A.alloc("cacc0", [P, 512], F32), A.alloc("cacc1", [P, 512], F32)]
            vt = [A.alloc("vt0", [P, 4, 257], BF16), A.alloc("vt1", [P, 4, 257], BF16)]
            kt = [A.alloc("kt0", [P, D], BF16), A.alloc("kt1", [P, D], BF16)]
            smT = [A.alloc("smT0", [P, 4, 128], BF16), A.alloc("smT1", [P, 4, 128], BF16)]
            gsb4 = A.alloc("gsb4", [P, 32], F32)
            ge4 = A.alloc("ge4", [P, 16], F32)
            gn4 = A.alloc("gn4", [P, 16], F32)
            gv4 = A.alloc("gv4", [P, 16], F32)
            gp4 = A.alloc("gp4", [P, 48], F32)
            gM4 = A.alloc("gM4", [P, 16], F32)
            gx4 = A.alloc("gx4", [P, 48], F32)
            mx16 = A.alloc("mx16", [P, 32], F32)
            hg = [A.alloc("hg0", [P, D], F32), A.alloc("hg1", [P, D], F32)]
            hn = A.alloc("hn", [P, D], BF16)
            bn2 = A.alloc("bn2", [P, 4, 6], F32)
            mv2 = A.alloc("mv2", [P, 4, 2], F32)
            rs2 = A.alloc("rs2", [P, 4], F32)
            nmr = A.alloc("nmr", [P, 4], F32)
            dcol = A.alloc("dcol", [P, 16], F32)
            e1 = [A.alloc("e1_0", [P, 128], F32), A.alloc("e1_1", [P, 128], F32)]
            e2 = [A.alloc("e2_0", [P, 128], F32), A.alloc("e2_1", [P, 128], F32)]
            if a == 0:
                print('phase DE end', A.off)
            cp("pool", xmT[:, :, 0:3], halo[:, :, 0:3], ["halo"], ["xm_halo"])
            for c in range(2):
                w, wn = w_cur(a, CK_XM0 + c)
                for jj in range(4):
                    j = c * 4 + jj
                    bi = jj % 2
                    ps, psn = rot()
                    proj_feat(ps, psn, w, wn, jj * 128)
                    cp("act", xmT[:, j, 3:515], ps, [psn], [("xmT", j)])
                    cp("pool", halo[:, j, 0:3], xmT[:, j, 512:515], [("xmT", j)], ["halo"])
                    ca, can = cacc[bi], ("cacc", bi)
                    ts("dve", ca[:], xmT[:, j, 0:512], convw[:, j, 0:1], None, ALU.mult, None,
                       [("xmT", j), "xm_halo", "convw"], [can])
                    for tap in range(1, 4):
                        stt(ca[:], xmT[:, j, tap:tap + 512], convw[:, j, tap:tap + 1], ca[:], ALU.mult, ALU.add,
                            [("xmT", j), "xm_halo", "convw", can], [can])
                    act(xcT[:, j, :], ca[:], AF.Silu, [can, "convb"], [("xcT", j)], bias=convb[:, j:j + 1])
                w_done()
            for c in range(2):
                w, wn = w_cur(a, CK_ZM0 + c)
                for jj in range(4):
                    j = c * 4 + jj
                    bi = jj % 2
                    ps, psn = rot()
                    proj_feat(ps, psn, w, wn, jj * 128)
                    act(zmT[:, j, :], ps, AF.Silu, [psn], [("zmT", j)])
                w_done()
            for c in range(2):
                w, wn = w_cur(a, CK_OM0 + c)
                for t in range(4):
                    bi = t % 2
                    ps, psn = rot()
                    proj_tok(ps, psn, w, wn, lambda k, t=t: hT[:, k, t * P:(t + 1) * P], [("hT", t)])
                    act(om[:, t, c * 512:(c + 1) * 512], ps, AF.Sigmoid, [psn], [("om", t, c)])
                w_done()
            xc_all = [("xcT", j) for j in range(8)]
            xm_all = [("xmT", j) for j in range(8)]
            for h in range(4):
                for ec in range(2):
                    for which, (wb, wbn, dst, dn, scl) in enumerate(((wq_b, "wq_b", qmT, "qmT", 1.0), (wk_b, "wk_b", kmT, "kmT", 1.0 / 16.0))):
                        bi = (ec + which) % 2
                        ps, psn = rot()
                        for dc in range(2):
                            mm(ps, wb[:, h, dc, ec * 128:(ec + 1) * 128], xcT[:, 2 * h + dc, :], dc == 0, dc == 1,
                               [wbn, ("xcT", 2 * h + dc)], [psn])
                        if scl == 1.0:
                            cp("act", dst[:, 2 * h + ec, :], ps, [psn], [(dn, 2 * h + ec)])
                        else:
                            S.op("act", lambda e, o=dst[:, 2 * h + ec, :], i=ps, s=scl: e.mul(o, i, s), reads=[psn], writes=[(dn, 2 * h + ec)])
            if a == 0:
                dump("xcT", xcT[:], xc_all, [P, 8, BLK], BF16)
                dump("qmT", qmT[:], [("qmT", j) for j in range(8)], [P, 8, BLK], BF16)
                dump("kmT", kmT[:], [("kmT", j) for j in range(8)], [P, 8, BLK], BF16)

            ptBf = ptB[:].bitcast(F32)
            for c in range(4):
                tokc = slice(c * P, (c + 1) * P)
                tokxc = slice(3 + c * P, 3 + (c + 1) * P)
                for j in range(8):
                    mm(PB[3][:, c * 8:(c + 1) * 8], xcT[:, j, tokc], AB[:, j, :], j == 0, False, [("xcT", j), "AB"], ["PB3"], inc=False)
                for j in range(8):
                    mm(PB[3][:, c * 8:(c + 1) * 8], xmT[:, j, tokxc], AB[:, 8 + j, :], False, j == 7, [("xmT", j), "AB"], ["PB3"], inc=(j == 7))
            g4 = gsb4[:].rearrange("p (c g) -> p c g", g=8)
            tt("dve", g4, PB[3][:, 0:32].rearrange("p (c g) -> p c g", g=8), bif_bc[:].unsqueeze(1).to_broadcast([P, 4, 8]),
               ALU.add, ["PB3", "bif_bc"], ["gsb4"])
            act(ge4[:].rearrange("p (c h) -> p c h", h=4), g4[:, :, 4:8], AF.Exp, ["gsb4"], ["ge4"], scale=-1.0)
            act(gn4[:], ge4[:], AF.Ln, ["ge4", "cols"], ["gn4"], bias=one_col)
            for c in range(4):
                mm(PB[3][:, 32 + c * 4:36 + c * 4], tri_f, gn4[:, c * 4:(c + 1) * 4], True, True, ["cmat", "gn4"], ["PB3"])
                mm(PB[3][:, 48 + c * 4:52 + c * 4], ones_f, gn4[:, c * 4:(c + 1) * 4], True, True, ["cmat", "gn4"], ["PB3"])
            tt("dve", gv4[:].rearrange("p (c h) -> p c h", h=4), g4[:, :, 0:4], PB[3][:, 32:48].rearrange("p (c h) -> p c h", h=4),
               ALU.add, ["gsb4", "PB3"], ["gv4"])
            tr(PB[3][0:16, 128:256], gv4[:], ident_f, ["gv4", "cmat"], ["PB3"])
            S.op("dve", lambda e, o=mx16[0:16, 0:1], i=PB[3][0:16, 128:256]: e.reduce_max(o, i, axis=AX.X), reads=["PB3"], writes=["mx16"])
            ts("dve", mx16[0:16, 16:32], ident_f[0:16, 0:16], mx16[0:16, 0:1], None, ALU.mult, None, ["mx16", "cmat"], ["mxd16"])
            mm(PB[3][:, 64:80], ones_f[0:16, :], mx16[0:16, 16:32], True, True, ["cmat", "mxd16"], ["PB3"])
            cp("dve", gp4[:], PB[3][:, 32:80], ["PB3"], ["gp4"])
            if a > 0:
                cp("dve", mp4[:, 0:4], mp4[:, 16:20], ["mp4"], ["mp4"])
            for c in range(4):
                tt("dve", gM4[:, c * 4:(c + 1) * 4], mp4[:, c * 4:(c + 1) * 4], gp4[:, 32 + c * 4:36 + c * 4], ALU.max, ["mp4", "gp4"], ["gM4"])
                tt("dve", mp4[:, (c + 1) * 4:(c + 2) * 4], gM4[:, c * 4:(c + 1) * 4], gp4[:, 16 + c * 4:20 + c * 4], ALU.subtract,
                   ["gM4", "gp4", "mp4"], ["mp4"])
            tt("dve", gx4[:, 0:16], mp4[:, 0:16], gM4[:], ALU.subtract, ["mp4", "gM4"], ["gx4a"])
            tt("dve", gx4[:, 16:32], gv4[:], gM4[:], ALU.subtract, ["gv4", "gM4"], ["gx4b"])
            tt("dve", gx4[:, 32:48], gp4[:, 0:16], gM4[:], ALU.subtract, ["gp4", "gM4"], ["gx4c"])
            act(gx4[:], gx4[:], AF.Exp, ["gx4a", "gx4b", "gx4c"], ["gx4"])
            if a == 0:
                dump("gx4", gx4[:], ["gx4"], [P, 48])

            def e_pre(t):
                tb = t % 2
                tok = slice(t * P, (t + 1) * P)
                tokx = slice(3 + t * P, 3 + (t + 1) * P)
                p_ = gx4[:, 16 + 4 * t:20 + 4 * t]
                for hp in range(2):
                    ps, psn = PB[4 + hp], PBN[4 + hp]
                    for hh in range(2):
                        h = 2 * hp + hh
                        for dc in range(2):
                            mm(ps[:, hh * 256:(hh + 1) * 256], xmT[:, 2 * h + dc, tokx], wv_b[:, h, dc, :], dc == 0, dc == 1,
                               [("xmT", 2 * h + dc), "wv_b"], [psn], inc=(dc == 1 and hh == 1))
                    for hh in range(2):
                        h = 2 * hp + hh
                        S.op("act", lambda e, o=vt[tb][:, h, 0:256], i=ps[:, hh * 256:(hh + 1) * 256], sc=p_[:, h:h + 1]:
                             e.activation(o, i, AF.Identity, scale=sc), reads=[psn, "gx4"], writes=[("vt", tb, h)])
                cp("dve", vt[tb][:, :, 256:257], p_.unsqueeze(2), ["gx4"], [("vt1", tb)])
                for hp in range(2):
                    ps, psn = PB[4 + hp], PBN[4 + hp]
                    for hh in range(2):
                        h = 2 * hp + hh
                        for dc in range(2):
                            mm(ps[:, hh * 256:(hh + 1) * 256], xcT[:, 2 * h + dc, tok], wk_b[:, h, dc, :], dc == 0, dc == 1,
                               [("xcT", 2 * h + dc), "wk_b"], [psn], inc=(dc == 1 and hh == 1))
                    S.op("act", lambda e, o=kt[tb][:, hp * 512:(hp + 1) * 512], i=ps: e.mul(o, i, 1.0 / 16.0), reads=[psn], writes=[("kt", tb, hp)])
                for h in range(4):
                    for ec in range(2):
                        mm(PB[2][:, h * 128:(h + 1) * 128], kmT[:, 2 * h + ec, tok], qmT[:, 2 * h + ec, tok], ec == 0, ec == 1,
                           [("kmT", 2 * h + ec), ("qmT", 2 * h + ec)], ["PB2"], inc=(ec == 1 and h == 3))
                tt("dve", smT[tb][:], PB[2].rearrange("p (h j) -> p h j", h=4), triU_b[:].unsqueeze(1).to_broadcast([P, 4, 128]),
                   ALU.mult, ["PB2", "triU_b"], [("smT", tb)])

            def e_state(t):
                tb = t % 2
                tok = slice(t * P, (t + 1) * P)
                sc_ = gx4[:, 4 * t:4 * t + 4]
                thr_ = gx4[:, 32 + 4 * t:36 + 4 * t]
                hgt = hg[tb]
                for h in range(4):
                    S.op("act", lambda e, o=Cb[:, 2 * h:2 * h + 2, :], i=Cst[:, 2 * h:2 * h + 2, :], sc=sc_[:, h:h + 1]:
                         e.activation(o, i, AF.Identity, scale=sc), reads=[("Cst", h), "gx4"], writes=[("Cb", h)])
                for h in range(4):
                    for ec in range(2):
                        pc, pcn = (PB[3], "PB3") if ec == 0 else (ptBf, "ptB")
                        mm(pc[:, 0:257], kt[tb][:, h * 256 + ec * 128:h * 256 + (ec + 1) * 128], vt[tb][:, h, :], True, True,
                           [("kt", tb, h // 2), ("vt", tb, h), ("vt1", tb)], [pcn])
                        stt(Cst[:, 2 * h + ec, :], Cst[:, 2 * h + ec, :], sc_[:, h:h + 1], pc[:, 0:257], ALU.mult, ALU.add,
                            [("Cst", h), ("Cb", h), "gx4", pcn], [("Cst", h)])
                for h in range(4):
                    ps, psn = PB[h % 2], PBN[h % 2]
                    for ec in range(2):
                        mm(ps[:, 0:257], qmT[:, 2 * h + ec, tok], Cb[:, 2 * h + ec, :], ec == 0, False,
                           [("qmT", 2 * h + ec), ("Cb", h)], [psn], inc=False)
                    mm(ps[:, 0:257], smT[tb][:, h, :], vt[tb][:, h, :], False, True, [("smT", tb), ("vt", tb, h), ("vt1", tb)], [psn])
                    ts("dve", dcol[:, 4 + h:5 + h], ps[:, 256:257], -1.0, None, ALU.mult, None, [psn], [("dcolA", h)])
                    tt("dve", dcol[:, h:h + 1], ps[:, 256:257], dcol[:, 4 + h:5 + h], ALU.max, [psn, ("dcolA", h)], [("dcolB", h)])
                    tt("dve", dcol[:, 8 + h:9 + h], dcol[:, h:h + 1], thr_[:, h:h + 1], ALU.max, [("dcolB", h), "gx4"], [("dcolC", h)])
                    S.op("dve", lambda e, o=dcol[:, 12 + h:13 + h], i=dcol[:, 8 + h:9 + h]: e.reciprocal(o, i), reads=[("dcolC", h)], writes=[("dcolD", h)])
                    stt(hgt[:, h * 256:(h + 1) * 256], ps[:, 0:256], dcol[:, 12 + h:13 + h], om[:, t, h * 256:(h + 1) * 256],
                        ALU.mult, ALU.mult, [psn, ("dcolD", h), ("om", t, h // 2)], [("hg", tb, h)])
            def e_h2(t):
                tb = t % 2
                tok = slice(t * P, (t + 1) * P)
                hgt = hg[tb]
                for h in range(4):
                    S.op("dve", lambda e, o=bn2[:, h, :], i=hgt[:, h * 256:(h + 1) * 256]: e.bn_stats(o, i),
                         reads=[("hg", tb, h)], writes=[("bn2", h)])
                    S.op("dve", lambda e, o=mv2[:, h, :], i=bn2[:, h, :]: e.bn_aggr(o, i), reads=[("bn2", h)], writes=[("mv2", h)])
                mv2r = [("mv2", h) for h in range(4)]
                act(rs2[:], mv2[:, :, 1], AF.Sqrt, mv2r + ["cols"], ["rs2"], bias=eps_col)
                S.op("dve", lambda e, o=rs2[:]: e.reciprocal(o, o), reads=["rs2"], writes=["rs2"])
                stt(nmr[:], mv2[:, :, 0], -1.0, rs2[:], ALU.mult, ALU.mult, mv2r + ["rs2"], ["nmr"])
                for h in range(4):
                    act(hn[:, h * 256:(h + 1) * 256], hgt[:, h * 256:(h + 1) * 256], AF.Identity, [("hg", tb, h), "rs2", "nmr"], [("hn", h)],
                        bias=nmr[:, h:h + 1], scale=rs2[:, h:h + 1])
                if a == 0 and t == 0:
                    dump("hn", hn[:], [("hn", h) for h in range(4)], [P, D], BF16)
                for j in range(8):
                    tr(ptA[:, j * 128:(j + 1) * 128], hn[:, j * 128:(j + 1) * 128], ident_b[:], [("hn", j // 2), "ident_b"], ["ptA"],
                       inc=(j == 7))
                for j in range(8):
                    bi = j % 2
                    S.op("act", lambda e, o=e1[bi][:], i=xcT[:, j, tok], sc=skipc[:, j:j + 1]: e.activation(o, i, AF.Identity, scale=sc),
                         reads=[("xcT", j), "skipc"], writes=[("e1", bi)])
                    stt(e2[bi][:], ptA[:, j * 128:(j + 1) * 128], mhw[:, j:j + 1], e1[bi][:], ALU.mult, ALU.add,
                        ["ptA", "mhw", ("e1", bi)], [("e2", bi)])
                    tt("dve", yminT[:, j, tok], e2[bi][:], zmT[:, j, tok], ALU.mult, [("e2", bi), ("zmT", j)], [("yminT", j)])

            e_pre(0)
            e_state(0)
            for t in range(4):
                if t < 3:
                    e_pre(t + 1)
                    e_state(t + 1)
                e_h2(t)
            if a == 0:
                dump("yminT", yminT[:], [("yminT", j) for j in range(8)], [P, 8, BLK], BF16)
            S.barrier()

            A.release(phase_mark)
            gaT = A.alloc("gaT", [P, 8, BLK], BF16)
            gmT = A.alloc("gmT", [P, 8, BLK], BF16)
            mT = A.alloc("mT", [P, 8, BLK], BF16)
            u1 = [A.alloc("u1_0", [P, 512], F32), A.alloc("u1_1", [P, 512], F32)]
            xt_n = [A.alloc("xtn0", [P, D], F32), A.alloc("xtn1", [P, D], F32)]
            bn_n = A.alloc("bn_n", [P, 2, 2, 6], F32)
            mv_n = A.alloc("mv_n", [P, 2, 2], F32)
            rstd_n = A.alloc("rstd_n", [P, 2], F32)
            fg_early_end = A.off
            res = A.alloc("res", [P, 4, D], F32)
            xr = [A.alloc("xr%d" % i, [P, D], F32) for i in range(4)]
            bn3 = A.alloc("bn3", [P, 4, 2, 6], F32)
            mv3 = A.alloc("mv3", [P, 4, 2], F32)
            rs3 = A.alloc("rs3", [P, 4], F32)
            nm3 = A.alloc("nm3", [P, 4], F32)
            assert b_end <= fg_early_end, (b_end, fg_early_end)
            for t in range(4):
                dma(xr[t][:], x_d[t0 + t * P:t0 + (t + 1) * P, :], [], [("xr", t)])
            if a == 0:
                print('phase FG end', A.off)
            for which, (dst, dn) in enumerate(((gaT, "gaT"), (gmT, "gmT"))):
                for c in range(2):
                    w, wn = w_cur(a, (CK_GA0 if which == 0 else CK_GM0) + c)
                    for jj in range(4):
                        j = c * 4 + jj
                        bi = jj % 2
                        ps, psn = rot()
                        proj_feat(ps, psn, w, wn, jj * 128)
                        act(dst[:, j, :], ps, AF.Sigmoid, [psn], [(dn, j)])
                    w_done()
            if a + 1 < NBLK:
                emit_ln(a + 1, xt_n, bn_n, mv_n, rstd_n)
            w, wn = w_cur(a, CK_PA)
            wpa = w[:].rearrange("p k n -> p (k n)").rearrange("p (k n) -> p k n", k=4)
            ogr = [("ogT", r) for r in range(4)]
            u1s = {}
            for j in range(8):
                bi = j % 2
                ps, psn = rot()
                for k in range(4):
                    mm(ps, wpa[:, k, j * 128:(j + 1) * 128], ogT[:, k, :], k == 0, k == 3, [wn] + ogr, [psn])
                tt("dve", mT[:, j, :], ps, gaT[:, j, :], ALU.mult, [psn, ("gaT", j)], [("mT", j)])
            w_done()
            for c in range(2):
                w, wn = w_cur(a, CK_PM0 + c)
                for jj in range(4):
                    j = c * 4 + jj
                    bi = jj % 2
                    ps, psn = rot()
                    for k in range(8):
                        mm(ps, w[:, k, jj * 128:(jj + 1) * 128], yminT[:, k, :], k == 0, k == 7,
                           [wn] + [("yminT", kk) for kk in range(8)], [psn])
                    tt("dve", u1[bi][:], ps, gmT[:, j, :], ALU.mult, [psn, ("gmT", j)], [("u1", bi)])
                    tt("dve", mT[:, j, :], u1[bi][:], mT[:, j, :], ALU.add, [("u1", bi), ("mT", j)], [("mT", j)])
                w_done()
            if a == 0:
                dump("mT", mT[:], [("mT", j) for j in range(8)], [P, 8, BLK], BF16)
            alpha = 2.0 ** 0.25
            for c in range(2):
                w, wn = w_cur(a, CK_OUT0 + c)
                for t in range(4):
                    bi = t % 2
                    ps, psn = rot()
                    for k in range(8):
                        mm(ps, mT[:, k, t * P:(t + 1) * P], w[:, k, :], k == 0, k == 7, [wn] + [("mT", kk) for kk in range(8)], [psn])
                    tt("dve", res[:, t, c * 512:(c + 1) * 512], ps, gate_bc[:, c * 512:(c + 1) * 512], ALU.mult,
                       [psn, "gate_bc"], [("res", t, c)])
                w_done()
            fence1 = S.snapshot()
            for t in range(4):
                xs, xn = xr[t], ("xr", t)
                stt(xs[:], xs[:], alpha, res[:, t, :], ALU.mult, ALU.add, [xn, ("res", t, 0), ("res", t, 1)], [xn])
            for t in range(4):
                xs, xn = xr[t], ("xr", t)
                for hf in range(2):
                    S.op("dve", lambda e, o=bn3[:, t, hf, :], i=xs[:, hf * 512:(hf + 1) * 512]: e.bn_stats(o, i),
                         reads=[xn], writes=[("bn3", t)])
                S.op("dve", lambda e, o=mv3[:, t, :], i=bn3[:, t].rearrange("p a b -> p (a b)"): e.bn_aggr(o, i),
                     reads=[("bn3", t)], writes=[("mv3", t)])
            mv3r = [("mv3", t) for t in range(4)]
            act(rs3[:], mv3[:, :, 1], AF.Sqrt, mv3r + ["cols"], ["rs3"], bias=eps_col)
            S.op("dve", lambda e, o=rs3[:]: e.reciprocal(o, o), reads=["rs3"], writes=["rs3"])
            stt(nm3[:], mv3[:, :, 0], -1.0, rs3[:], ALU.mult, ALU.mult, mv3r + ["rs3"], ["nm3"])
            for t in range(4):
                xs, xn = xr[t], ("xr", t)
                act(xs[:], xs[:], AF.Identity, [xn, "rs3", "nm3"], [xn], bias=nm3[:, t:t + 1], scale=rs3[:, t:t + 1])
            for t in range(4):
                xs, xn = xr[t], ("xr", t)
                tt("dve", xs[:], xs[:], lng_bc[:], ALU.mult, [xn, "lng_bc"], [xn])
                tt("dve", xs[:], xs[:], lnb_bc[:], ALU.add, [xn, "lnb_bc"], [xn])
                dma(y_d[t0 + t * P:t0 + (t + 1) * P, :], xs[:], [xn], [])
            fence2 = S.snapshot()

        S.finish()
        S.emit()
    print("op counts", S.cnt, S.dma_cnt)
    print("SBUF arena peak", A.peak, "of", SBUF_TOP, "persistent end", phase_mark)
    return nc, dumps


def _consts():
    i = np.arange(128)[:, None]
    p = np.arange(128)[None, :]
    mg0 = np.zeros((128, 4, 5, 128), np.float32)
    for r in range(4):
        for j in range(5):
            d = (4 * i + r) - (128 * (j - 1) + p)
            mg0[:, r, j, :] = np.where((d >= 0) & (d <= 128), 0.0, NEG)
    mg0 = mg0.reshape(128, 4, 640)
    g1_prev = np.where(p >= i, 0.0, NEG)
    g1_own = np.where(p <= i, 0.0, NEG)
    same = ((i - p) % 4 == 0)
    g2_far = np.where(same & (p >= i), 0.0, NEG)
    g2_mid = np.where(same, 0.0, NEG)
    g2_own = np.where(same & (p <= i), 0.0, NEG)
    mg12 = np.concatenate([g1_prev, g1_own, g2_far, g2_mid, g2_mid, g2_mid, g2_own], axis=1).astype(np.float32)
    cmat = np.zeros((128, 4, 128), np.float32)
    cmat[:, 0, :] = np.eye(128)
    cmat[:, 1, :] = 1.0
    cmat[:, 2, :] = (i <= p)
    invf = np.power(np.float32(10000.0), -np.arange(64, dtype=np.float32) / np.float32(64)).astype(np.float32)
    invf = np.broadcast_to(invf[None, :], (128, 64)).copy()
    return mg0, mg12, cmat, invf


def make_in_maps(inputs, S_LEN=4096, n_cores=8):
    f = lambda a: np.ascontiguousarray(np.asarray(a, dtype=np.float32))
    x = f(inputs["x"])
    c = f(inputs["c"])
    pos = np.asarray(inputs["positions"]).astype(np.int32)
    mg0, mg12, cmat, invf = _consts()
    col = lambda v: np.ascontiguousarray(v.reshape(-1, 128).T)
    shared = {
        "w_ada": f(inputs["w_ada"][0]),
        "b_ada_col": col(f(inputs["b_ada"][0])),
        "b_ada": f(inputs["b_ada"][0]).reshape(1, -1),
        "w_in": f(inputs["w_in"][0]),
        "convw_col": np.ascontiguousarray(f(inputs["conv_w"][0]).T.reshape(8, 128, 4).transpose(1, 0, 2)),
        "convb_col": col(f(inputs["conv_b"][0])),
        "w_qm": f(inputs["w_qm"][0]), "w_km": f(inputs["w_km"][0]), "w_vm": f(inputs["w_vm"][0]),
        "w_qmT": np.ascontiguousarray(f(inputs["w_qm"][0]).transpose(0, 2, 1)),
        "w_kmT": np.ascontiguousarray(f(inputs["w_km"][0]).transpose(0, 2, 1)),
        "w_vmT": np.ascontiguousarray(f(inputs["w_vm"][0]).transpose(0, 2, 1)),
        "w_if": f(inputs["w_if"][0]),
        "b_if": f(inputs["b_if"][0]).reshape(1, 8),
        "mhw_col": col(f(inputs["mh_norm_w"][0])),
        "skip_col": col(f(inputs["skip_m"][0])),
        "w_pa": f(inputs["w_pa"][0]), "w_pm": f(inputs["w_pm"][0]), "w_out": f(inputs["w_out"][0]),
        "ln_g": f(inputs["ln_g"][0]).reshape(1, -1), "ln_b": f(inputs["ln_b"][0]).reshape(1, -1),
        "invf": invf, "mg0": mg0, "mg12": mg12, "cmat": cmat,
    }
    maps = []
    for b in range(n_cores):
        m = dict(shared)
        m["x"] = np.ascontiguousarray(x[b, :S_LEN])
        m["cT"] = col(c[b])
        m["posT"] = np.ascontiguousarray(pos[b, :S_LEN].reshape(-1, 128).T)
        maps.append(m)
    return maps


_NC_CACHE = {}


def kernel(**inputs):
    if "nc" not in _NC_CACHE:
        _NC_CACHE["nc"] = build_nc(4096, dbg=False)[0]
    nc = _NC_CACHE["nc"]
    maps = make_in_maps(inputs, 4096, 8)
    res = run_bass_kernel_spmd(nc, maps, core_ids=list(range(8)))
    out = np.stack([np.asarray(r["y"], dtype=np.float32) for r in res.results], axis=0)
    return out
```

```python
import numpy as np
from contextlib import ExitStack
import concourse.bass as bass
import concourse.mybir as mybir
from concourse.bass_utils import run_bass_kernel_spmd

F32 = mybir.dt.float32
BF16 = mybir.dt.bfloat16
I32 = mybir.dt.int32
AF = mybir.ActivationFunctionType
ALU = mybir.AluOpType
AX = mybir.AxisListType

ENGS = ("pe", "act", "dve", "pool", "sp")
NDMA = 14
import os
EMBED = bool(int(os.environ.get("K_EMBED", "0")))
_st = os.environ.get("K_STRICT", "none")
STRICT = set(ENGS) if _st == "all" else set(x for x in _st.split(",") if x)
NDMA_SP = 8
P = 128
D = 1024
BLK = 512
NEG = -30000.0
PI_LO = 3.1415925
TWO_PI = 6.2831853
SBUF_BASE = 16512
SBUF_TOP = 229376


class Sched:
    def __init__(self, nc):
        self.nc = nc
        self.ops = {e: [] for e in ENGS}
        self.cnt = {e: 0 for e in ENGS}
        self.known = {e: {} for e in ENGS}
        self.lastw = {}
        self.rd = {}
        self.dma_cnt = [0] * NDMA
        self.dma_rr_sp = 0
        self.dma_rr_pl = 0

    def _deps(self, eng, reads, writes, extra=()):
        deps = {}

        def add(tok):
            if tok is None:
                return
            k, v = tok
            if deps.get(k, 0) < v:
                deps[k] = v

        for t in extra:
            add(t)
        for r in reads:
            add(self.lastw.get(r))
        for w in writes:
            add(self.lastw.get(w))
            for k, v in self.rd.get(w, {}).items():
                add((k, v))
        waits = []
        for k, v in deps.items():
            if k == "pe" and eng == "pe":
                continue
            if self.known[eng].get(k, 0) >= v:
                continue
            self.known[eng][k] = v
            waits.append((k, v))
        return waits

    def _mark(self, tok, reads, writes):
        k, v = tok
        for r in reads:
            d = self.rd.setdefault(r, {})
            if d.get(k, 0) < v:
                d[k] = v
        for w in writes:
            self.lastw[w] = tok
            self.rd[w] = {}

    def op(self, eng, fn, reads=(), writes=(), inc=True):
        assert inc or eng == "pe"
        extra = self._all_tokens(sp_only=True) if eng in STRICT else ()
        waits = self._deps(eng, reads, writes, extra)
        if inc:
            self.cnt[eng] += 1
            tok = (eng, self.cnt[eng])
        else:
            tok = (eng, self.cnt[eng] + 1)
        self._mark(tok, reads, writes)
        self.ops[eng].append((fn, waits, eng if inc else None))

    def dma(self, fn, reads=(), writes=(), queue="sp"):
        if queue == "sp":
            i = self.dma_rr_sp
            self.dma_rr_sp = (i + 1) % NDMA_SP
        else:
            i = NDMA_SP + self.dma_rr_pl
            self.dma_rr_pl = (self.dma_rr_pl + 1) % (NDMA - NDMA_SP)
        key = ("dma", i)
        prev = self.dma_cnt[i]
        extra = [(key, prev)] if prev else []
        if queue in STRICT or (queue == "pool" and "poolq" in STRICT):
            extra = extra + self._all_tokens(sp_only=True)
        waits = self._deps(queue, reads, writes, extra)
        self.dma_cnt[i] = prev + 16
        tok = (key, prev + 16)
        self._mark(tok, reads, writes)
        self.ops[queue].append((fn, waits, key))

    def _all_tokens(self, sp_only=False):
        toks = []
        for i in range(NDMA_SP if sp_only else NDMA):
            if self.dma_cnt[i]:
                toks.append((("dma", i), self.dma_cnt[i]))
        for e in ENGS:
            if e != "sp" and self.cnt[e]:
                toks.append((e, self.cnt[e]))
        return toks

    def snapshot(self):
        return self._all_tokens(sp_only=True)

    def barrier(self):
        self.wait_tokens(self._all_tokens(sp_only=True))

    def wait_tokens(self, toks):
        for e in ("pe", "act", "dve", "pool", "sp"):
            waits = []
            for k, v in toks:
                if k == e or (e == "sp" and isinstance(k, tuple)):
                    continue
                if self.known[e].get(k, 0) >= v:
                    continue
                self.known[e][k] = v
                waits.append((k, v))
            self.ops[e].append((None, waits, None))

    def finish(self):
        self.ops["sp"].append((None, self._all_tokens(), None))

    def emit(self):
        nc = self.nc
        with ExitStack() as st:
            sems = {}
            for e in ENGS:
                sems[e] = st.enter_context(nc.semaphore("s_" + e))
            for i in range(NDMA):
                sems[("dma", i)] = st.enter_context(nc.semaphore("s_dma%d" % i))
            block = st.enter_context(nc.Block())

            def run(eng, e):
                for fn, waits, inc in self.ops[e]:
                    emb = None
                    if EMBED and fn is not None and waits and e == "pe":
                        emb = waits[-1]
                        waits = waits[:-1]
                    for k, v in waits:
                        eng.wait_ge(sems[k], v)
                    if fn is None:
                        continue
                    ins = fn(eng)
                    if emb is not None:
                        ins._wait_ge(sems[emb[0]], emb[1])
                    if inc is not None:
                        ins.then_inc(sems[inc], 16 if isinstance(inc, tuple) else 1)

            @block.tensor
            def _(eng):
                run(eng, "pe")

            @block.scalar
            def _(eng):
                run(eng, "act")

            @block.vector
            def _(eng):
                run(eng, "dve")

            @block.gpsimd
            def _(eng):
                run(eng, "pool")

            @block.sync
            def _(eng):
                run(eng, "sp")


def _dsize(dt):
    return 4 if dt in (F32, I32) else 2


class Arena:
    def __init__(self, nc):
        self.nc = nc
        self.off = SBUF_BASE
        self.n = 0
        self.peak = SBUF_BASE

    def alloc(self, name, shape, dt):
        nbytes = int(np.prod(shape[1:])) * _dsize(dt)
        nbytes = (nbytes + 31) // 32 * 32
        assert self.off + nbytes <= SBUF_TOP, (name, self.off, nbytes)
        self.n += 1
        t = self.nc.alloc_sbuf_tensor_at("%s_%d" % (name, self.n), list(shape), dt, offset=self.off)
        self.off += nbytes
        self.peak = max(self.peak, self.off)
        return t

    def mark(self):
        return self.off

    def release(self, m):
        self.off = m


CK_Q0, CK_Q1, CK_Q2, CK_K, CK_V, CK_ZA, CK_XM0, CK_XM1, CK_ZM0, CK_ZM1, CK_OM0, CK_OM1, \
    CK_GA0, CK_GA1, CK_GM0, CK_GM1, CK_PA, CK_PM0, CK_PM1, CK_OUT0, CK_OUT1 = range(21)
NCHUNK = 21
STREAM_ORDER = [CK_K, CK_V, CK_Q0, CK_Q1, CK_Q2, CK_ZA, CK_XM0, CK_XM1, CK_ZM0, CK_ZM1,
                CK_OM0, CK_OM1, CK_GA0, CK_GA1, CK_GM0, CK_GM1, CK_PA, CK_PM0, CK_PM1,
                CK_OUT0, CK_OUT1]


def build_nc(S_LEN=4096, dbg=False):
    NBLK = S_LEN // BLK
    NTILE = S_LEN // P
    nc = bass.Bass("TRN2", target_bir_lowering=False)

    def din(name, shape, dt=F32):
        return nc.dram_tensor(name, list(shape), dt, kind="ExternalInput").ap()

    x_d = din("x", [S_LEN, D])
    cT_d = din("cT", [P, 8])
    pos_d = din("posT", [P, NTILE], I32)
    w_ada_d = din("w_ada", [D, 3 * D])
    b_ada_col_d = din("b_ada_col", [P, 24])
    b_ada_d = din("b_ada", [1, 3 * D])
    w_in_d = din("w_in", [D, 8192])
    convw_d = din("convw_col", [P, 8, 4])
    convb_d = din("convb_col", [P, 8])
    w_qm_d = din("w_qm", [4, 256, 256])
    w_km_d = din("w_km", [4, 256, 256])
    w_vm_d = din("w_vm", [4, 256, 256])
    w_qmT_d = din("w_qmT", [4, 256, 256])
    w_kmT_d = din("w_kmT", [4, 256, 256])
    w_vmT_d = din("w_vmT", [4, 256, 256])
    w_if_d = din("w_if", [3072, 8])
    b_if_d = din("b_if", [1, 8])
    mhw_d = din("mhw_col", [P, 8])
    skip_d = din("skip_col", [P, 8])
    w_pa_d = din("w_pa", [512, D])
    w_pm_d = din("w_pm", [D, D])
    w_out_d = din("w_out", [D, D])
    ln_g_d = din("ln_g", [1, D])
    ln_b_d = din("ln_b", [1, D])
    invf_d = din("invf", [P, 64])
    mg0_d = din("mg0", [P, 4, 640])
    mg12_d = din("mg12", [P, 896])
    cmat_d = din("cmat", [P, 4, 128])
    y_d = nc.dram_tensor("y", [S_LEN, D], F32, kind="ExternalOutput").ap()
    wbf_d = nc.dram_tensor("wbf", [NCHUNK, P, 4096], BF16).ap()
    dumps = {}

    def dump_decl(name, shape, dt=F32):
        dumps[name] = nc.dram_tensor("dbg_" + name, list(shape), dt, kind="ExternalOutput").ap()
        return dumps[name]

    S = Sched(nc)
    A = Arena(nc)
    with ExitStack() as st:
        ps_s = st.enter_context(nc.psum_tensor("ps_s", [P, 1536], F32))
        pb3 = st.enter_context(nc.psum_tensor("pb3", [P, 512], F32))
        pb4 = st.enter_context(nc.psum_tensor("pb4", [P, 512], F32))
        pb5 = st.enter_context(nc.psum_tensor("pb5", [P, 512], F32))
        ptA = st.enter_context(nc.psum_tensor("ptA", [P, 1024], BF16))
        ptB = st.enter_context(nc.psum_tensor("ptB", [P, 1024], BF16))
        PB = [ps_s[:, 0:512], ps_s[:, 512:1024], ps_s[:, 1024:1536], pb3[:], pb4[:], pb5[:]]
        PBN = ["PB0", "PB1", "PB2", "PB3", "PB4", "PB5"]
        PS_S = ["PB0", "PB1", "PB2"]

        cmat = A.alloc("cmat", [P, 4, 128], F32)
        ident_f = cmat[:, 0, :]
        ones_f = cmat[:, 1, :]
        tri_f = cmat[:, 2, :]
        ident_b = A.alloc("ident_b", [P, 128], BF16)
        triU_b = A.alloc("triU_b", [P, 128], BF16)
        mg0 = A.alloc("mg0", [P, 4, 640], BF16)
        mg12 = A.alloc("mg12", [P, 896], BF16)
        invf = A.alloc("invf", [P, 64], F32)
        pos_i = A.alloc("pos_i", [P, NTILE], I32)
        pos_f = A.alloc("pos_f", [P, NTILE], F32)
        cols = A.alloc("cols", [P, 8], F32)
        pi_col = cols[:, 0:1]
        eps_col = cols[:, 1:2]
        one_col = cols[:, 2:3]
        cT = A.alloc("cT", [P, 8], F32)
        siluc = A.alloc("siluc", [P, 8], F32)
        sc1 = A.alloc("sc1", [P, 8], F32)
        sh = A.alloc("sh", [P, 8], F32)
        badac = A.alloc("badac", [P, 24], F32)
        gate_bc = A.alloc("gate_bc", [P, D], F32)
        lng_bc = A.alloc("lng_bc", [P, D], F32)
        lnb_bc = A.alloc("lnb_bc", [P, D], F32)
        mhw = A.alloc("mhw", [P, 8], F32)
        skipc = A.alloc("skipc", [P, 8], F32)
        convw = A.alloc("convw", [P, 8, 4], F32)
        convb = A.alloc("convb", [P, 8], F32)
        bif_bc = A.alloc("bif_bc", [P, 8], F32)
        AB = A.alloc("AB", [P, 16, 8], BF16)
        wq_b = A.alloc("wq_b", [P, 4, 2, 256], BF16)
        wk_b = A.alloc("wk_b", [P, 4, 2, 256], BF16)
        wv_b = A.alloc("wv_b", [P, 4, 2, 256], BF16)
        KT = A.alloc("KT", [P, 4, 5 * BLK], BF16)
        V1 = A.alloc("V1", [P, 20, 512], BF16)
        V0 = A.alloc("V0", [P, 8, 512], BF16)
        Cst = A.alloc("Cst", [P, 8, 257], F32)
        Cb = A.alloc("Cb", [P, 8, 257], BF16)
        mp4 = A.alloc("mp4", [P, 20], F32)
        halo = A.alloc("halo", [P, 8, 4], BF16)
        wst = [A.alloc("wst0", [P, 8, 512], BF16), A.alloc("wst1", [P, 8, 512], BF16)]
        hT = A.alloc("hT", [P, 8, BLK], BF16)
        ogT = A.alloc("ogT", [P, 4, BLK], BF16)
        yminT = A.alloc("yminT", [P, 8, BLK], BF16)
        phase_mark = A.mark()

        def mm(out, lhsT, rhs, start, stop, reads, writes, inc=None):
            if inc is None:
                inc = stop
            S.op("pe", lambda e: e.matmul(out, lhsT, rhs, start=start, stop=stop),
                 reads=reads, writes=writes, inc=inc)

        def tr(out, in_, ident, reads, writes, inc=True):
            S.op("pe", lambda e: e.transpose(out, in_, ident), reads=reads, writes=writes, inc=inc)

        def act(out, in_, func, reads, writes, bias=None, scale=1.0, accum=None):
            kw = {}
            if bias is not None:
                kw["bias"] = bias
            if accum is not None:
                kw["accum_out"] = accum
            S.op("act", lambda e: e.activation(out, in_, func, scale=scale, **kw),
                 reads=reads, writes=writes)

        def cp(eng, out, in_, reads, writes):
            if eng == "act":
                S.op("act", lambda e: e.copy(out, in_), reads=reads, writes=writes)
            else:
                S.op(eng, lambda e: e.tensor_copy(out, in_), reads=reads, writes=writes)

        def tt(eng, out, a, b, op, reads, writes):
            S.op(eng, lambda e: e.tensor_tensor(out, a, b, op), reads=reads, writes=writes)

        def ts(eng, out, a, s1, s2, op0, op1, reads, writes):
            if s2 is None:
                S.op(eng, lambda e: e.tensor_scalar(out, a, s1, None, op0), reads=reads, writes=writes)
            else:
                S.op(eng, lambda e: e.tensor_scalar(out, a, s1, s2, op0, op1), reads=reads, writes=writes)

        def stt(out, a, s, b, op0, op1, reads, writes):
            S.op("dve", lambda e: e.scalar_tensor_tensor(out, a, s, b, op0, op1), reads=reads, writes=writes)

        def dma(out, in_, reads, writes, queue="sp"):
            S.dma(lambda e: e.dma_start(out=out, in_=in_), reads=reads, writes=writes, queue=queue)

        def dump(name, sb_ap, reads, shape, dt=F32):
            if not dbg:
                return
            d = dump_decl(name, shape, dt)
            dma(d, sb_ap, reads, [])

        def cast_chunk(ck):
            if ck <= CK_GM1:
                src = w_in_d[:, ck * 512:(ck + 1) * 512].rearrange("(k p) n -> p k n", p=P)
                dst = wbf_d[ck].rearrange("p (k n) -> p k n", k=8)
            elif ck == CK_PA:
                src = w_pa_d.rearrange("(k p) n -> p k n", p=P)
                dst = wbf_d[ck].rearrange("p (k n) -> p k n", k=4)
            elif ck in (CK_PM0, CK_PM1):
                c = ck - CK_PM0
                src = w_pm_d[:, c * 512:(c + 1) * 512].rearrange("(k p) n -> p k n", p=P)
                dst = wbf_d[ck].rearrange("p (k n) -> p k n", k=8)
            else:
                c = ck - CK_OUT0
                src = w_out_d[:, c * 512:(c + 1) * 512].rearrange("(k p) n -> p k n", p=P)
                dst = wbf_d[ck].rearrange("p (k n) -> p k n", k=8)
            dma(dst, src, [], [("wbf", ck)], queue="pool")

        dma(cmat[:], cmat_d, [], ["cmat"])
        dma(invf[:], invf_d, [], ["invf"])
        dma(pos_i[:], pos_d, [], ["pos_i"])
        dma(cT[:], cT_d, [], ["cT"])
        dma(badac[:], b_ada_col_d, [], ["badac"])
        dma(mhw[:], mhw_d, [], ["mhw"])
        dma(skipc[:], skip_d, [], ["skipc"])
        dma(convw[:], convw_d, [], ["convw"])
        dma(convb[:], convb_d, [], ["convb"])
        dma(bif_bc[:], b_if_d[0:1, :].partition_broadcast(P), [], ["bif_bc"])
        dma(lng_bc[:], ln_g_d[0:1, :].partition_broadcast(P), [], ["lng_bc"])
        dma(lnb_bc[:], ln_b_d[0:1, :].partition_broadcast(P), [], ["lnb_bc"])
        dma(gate_bc[:], b_ada_d[0:1, 2 * D:3 * D].partition_broadcast(P), [], ["gate_bc"])
        dma(mg0[:], mg0_d, [], ["mg0"], queue="pool")
        dma(mg12[:], mg12_d, [], ["mg12"], queue="pool")
        dma(wq_b[:], w_qm_d.rearrange("h (c p) e -> p h c e", p=P), [], ["wq_b"], queue="pool")
        dma(wk_b[:], w_km_d.rearrange("h (c p) e -> p h c e", p=P), [], ["wk_b"], queue="pool")
        dma(wv_b[:], w_vm_d.rearrange("h (c p) e -> p h c e", p=P), [], ["wv_b"], queue="pool")
        for ck in STREAM_ORDER:
            cast_chunk(ck)

        S.op("pool", lambda e: e.memset(cols[:, 0:1], PI_LO), writes=["cols"])
        S.op("pool", lambda e: e.memset(cols[:, 1:2], 1e-5), writes=["cols"])
        S.op("pool", lambda e: e.memset(cols[:, 2:3], 1.0), writes=["cols"])
        cp("dve", ident_b[:], ident_f, ["cmat"], ["ident_b"])
        cp("dve", triU_b[:], tri_f, ["cmat"], ["triU_b"])
        cp("dve", pos_f[:], pos_i[:], ["pos_i"], ["pos_f"])
        S.op("pool", lambda e: e.memset(Cst[:], 0.0), writes=["Cst"])
        S.op("pool", lambda e: e.memset(mp4[:], 0.0), writes=["mp4"])
        S.op("pool", lambda e: e.memset(halo[:], 0.0), writes=["halo"])

        pm = A.mark()
        stg = [A.alloc("stg0", [P, 8, 512], F32), A.alloc("stg1", [P, 8, 512], F32)]
        sbc = A.alloc("sbc", [P, 8, 128], F32)
        act(siluc[:], cT[:], AF.Silu, ["cT"], ["siluc"])
        for k in range(8):
            ts("dve", sbc[:, k, :], ones_f, siluc[:, k:k + 1], None, ALU.mult, None,
               ["cmat", "siluc"], [("sbc", k)])
        for piece in range(6):
            sgi = piece % 2
            dma(stg[sgi][:], w_ada_d[:, piece * 512:(piece + 1) * 512].rearrange("(k p) n -> p k n", p=P),
                [], [("stg", sgi)])
            if piece < 4:
                for jj in range(4):
                    j = piece * 4 + jj
                    for k in range(8):
                        mm(PB[3][:, j:j + 1], stg[sgi][:, k, jj * 128:(jj + 1) * 128], siluc[:, k:k + 1],
                           k == 0, k == 7, [("stg", sgi), "siluc"], ["PB3"])
            else:
                g = piece - 4
                for k in range(8):
                    mm(PB[4 + g], sbc[:, k, :], stg[sgi][:, k, :], k == 0, k == 7,
                       [("stg", sgi)] + [("sbc", kk) for kk in range(8)], [PBN[4 + g]])
                tt("dve", gate_bc[:, g * 512:(g + 1) * 512], PB[4 + g], gate_bc[:, g * 512:(g + 1) * 512], ALU.add,
                   [PBN[4 + g], "gate_bc"], ["gate_bc"])
        tt("dve", sh[:], PB[3][:, 0:8], badac[:, 0:8], ALU.add, ["PB3", "badac"], ["sh"])
        tt("dve", sc1[:], PB[3][:, 8:16], badac[:, 8:16], ALU.add, ["PB3", "badac"], ["sc1"])
        ts("dve", sc1[:], sc1[:], 1.0, None, ALU.add, None, ["sc1"], ["sc1"])

        A.release(pm)
        wif_s = A.alloc("wif_s", [P, 24, 8], F32)
        wT_s = A.alloc("wT_s", [P, 3, 4, 2, 256], F32)
        alias_w = [("stg", 0), ("stg", 1)] + [("sbc", k) for k in range(8)]
        dma(wif_s[:], w_if_d.rearrange("(c p) g -> p c g", p=P), [], ["wif_s"] + alias_w)
        for mi, wd in enumerate((w_qmT_d, w_kmT_d, w_vmT_d)):
            dma(wT_s[:, mi], wd.rearrange("h (c p) d -> p h c d", p=P), [], [("wT_s", mi)] + alias_w)
        ts("dve", wif_s[:, 8:16, :], wif_s[:, 8:16, :], 1.0 / 16.0, None, ALU.mult, None, ["wif_s"], ["wif_s"])
        for h in range(4):
            for dc in range(2):
                j = 2 * h + dc
                terms = [(0, 0 + 2 * h + ec, ec) for ec in range(2)] + [(1, 8 + 2 * h + ec, ec) for ec in range(2)]
                for ti, (mi, wc, ec) in enumerate(terms):
                    mm(PB[3][:, 32 + j * 8:32 + (j + 1) * 8], wT_s[:, mi, h, ec, dc * 128:(dc + 1) * 128], wif_s[:, wc, :],
                       ti == 0, ti == 3, [("wT_s", mi), "wif_s"], ["PB3"])
                for ec in range(2):
                    mm(PB[3][:, 96 + j * 8:96 + (j + 1) * 8], wT_s[:, 2, h, ec, dc * 128:(dc + 1) * 128],
                       wif_s[:, 16 + 2 * h + ec, :], ec == 0, ec == 1, [("wT_s", 2), "wif_s"], ["PB3"])
        cp("dve", AB[:, 0:8, :], PB[3][:, 32:96].rearrange("p (j g) -> p j g", g=8), ["PB3"], ["AB"])
        cp("dve", AB[:, 8:16, :], PB[3][:, 96:160].rearrange("p (j g) -> p j g", g=8), ["PB3"], ["AB"])
        S.barrier()
        A.release(phase_mark)

        seq = [(a, ck) for a in range(NBLK) for ck in STREAM_ORDER]
        wstate = {"next_load": 0, "cur": 0}

        def w_issue():
            i = wstate["next_load"]
            if i >= len(seq):
                return
            ck = seq[i][1]
            slot = i % 2
            dma(wst[slot][:].rearrange("p k n -> p (k n)"), wbf_d[ck], [("wbf", ck)], [("wst", slot)])
            wstate["next_load"] = i + 1

        def w_cur(a, ck):
            i = wstate["cur"]
            assert seq[i] == (a, ck), (seq[i], a, ck)
            return wst[i % 2], ("wst", i % 2)

        def w_done():
            wstate["cur"] += 1
            w_issue()

        w_issue()
        w_issue()

        hT_all = [("hT", t) for t in range(4)]
        rstate = {"i": 0}

        def rot():
            i = rstate["i"]
            rstate["i"] = (i + 1) % 6
            return PB[i], PBN[i]

        def proj_tok(ps, psn, w, wn, col_ap_fn, hreads, ncols=512, c0=0):
            for k in range(8):
                mm(ps, col_ap_fn(k), w[:, k, c0:c0 + ncols], k == 0, k == 7, hreads + [wn], [psn])

        def proj_feat(ps, psn, w, wn, c0):
            for k in range(8):
                mm(ps, w[:, k, c0:c0 + 128], hT[:, k, :], k == 0, k == 7, hT_all + [wn], [psn])

        def ln_a(ab, xt, bn, mv, rstd, nm):
            t0 = ab * BLK
            for t in range(4):
                dma(xt[t][:], x_d[t0 + t * P:t0 + (t + 1) * P, :], [], [("xt", t)])
            for t in range(4):
                xs, xn = xt[t], ("xt", t)
                for hf in range(2):
                    S.op("dve", lambda e, o=bn[:, t, hf, :], i=xs[:, hf * 512:(hf + 1) * 512]: e.bn_stats(o, i),
                         reads=[xn], writes=[("bn", t)])
                S.op("dve", lambda e, o=mv[:, t, :], i=bn[:, t].rearrange("p a b -> p (a b)"): e.bn_aggr(o, i),
                     reads=[("bn", t)], writes=[("mv", t)])
            mvr = [("mv", t) for t in range(4)]
            act(rstd[:], mv[:, :, 1], AF.Sqrt, mvr + ["cols"], ["rstd"], bias=eps_col)
            S.op("dve", lambda e, o=rstd[:]: e.reciprocal(o, o), reads=["rstd"], writes=["rstd"])
            stt(nm[:], mv[:, :, 0], -1.0, rstd[:], ALU.mult, ALU.mult, mvr + ["rstd"], ["lnnm"])
            for t in range(4):
                xs, xn = xt[t], ("xt", t)
                act(xs[:], xs[:], AF.Identity, [xn, "rstd", "lnnm"], [xn], bias=nm[:, t:t + 1], scale=rstd[:, t:t + 1])

        def ln_b(ab, xt):
            for t in range(4):
                xs, xn = xt[t], ("xt", t)
                for half in range(2):
                    pb, pbn = rot()
                    for kk in range(4):
                        k = half * 4 + kk
                        tr(pb[:, kk * 128:(kk + 1) * 128], xs[:, k * 128:(k + 1) * 128], ident_f,
                           [xn, "cmat"], [pbn], inc=(kk == 3))
                    for kk in range(4):
                        k = half * 4 + kk
                        act(hT[:, k, t * P:(t + 1) * P], pb[:, kk * 128:(kk + 1) * 128], AF.Identity,
                            [pbn, "sc1", "sh"], [("hT", t)], bias=sh[:, k:k + 1], scale=sc1[:, k:k + 1])

        for a in range(NBLK):
            t0 = a * BLK
            A.release(phase_mark)
            if a > 0:
                S.wait_tokens(fence1)
            t1 = [A.alloc("t1_0", [P, 512], F32), A.alloc("t1_1", [P, 512], F32)]
            t2 = [A.alloc("t2_0", [P, 512], F32), A.alloc("t2_1", [P, 512], F32)]
            qr = [A.alloc("qr0", [P, 512], BF16), A.alloc("qr1", [P, 512], BF16)]
            QT = A.alloc("QT", [P, 12, BLK], BF16)
            zs = A.alloc("zs", [P, 4, 512], BF16)
            cosT = A.alloc("cosT", [P, 4, 64], F32)
            sinT = A.alloc("sinT", [P, 4, 64], F32)
            angT = A.alloc("angT", [P, 4, 64], F32)
            angT2 = A.alloc("angT2", [P, 4, 64], F32)
            angQ = A.alloc("angQ", [P, 4, 64], F32)
            angI = A.alloc("angI", [P, 4, 64], I32)
            b_end = A.off
            if a == 0:
                m0 = A.mark()
                xt = [A.alloc("xt%d" % i, [P, D], F32) for i in range(4)]
                bn = A.alloc("bn", [P, 4, 2, 6], F32)
                mv = A.alloc("mv", [P, 4, 2], F32)
                rstd = A.alloc("rstd", [P, 4], F32)
                lnnm = A.alloc("lnnm", [P, 4], F32)
                A.release(m0)
            sm = [A.alloc("sm%d" % i, [P, 1536], F32) for i in range(3)]
            Pb = [A.alloc("Pb%d" % i, [P, 1536], BF16) for i in range(3)]
            PT = [A.alloc("PT0", [P, 12, 128], BF16), A.alloc("PT1", [P, 12, 128], BF16)]
            stat = A.alloc("stat", [P, 32], F32)
            og = A.alloc("og", [P, 512], BF16)

            tt("dve", angT[:], pos_f[:, 4 * a:4 * a + 4].unsqueeze(2).to_broadcast([P, 4, 64]),
               invf[:].unsqueeze(1).to_broadcast([P, 4, 64]), ALU.mult, ["pos_f", "invf"], ["angT"])
            INV2PI = 1.0 / 6.283185307179586
            for (dst, off, nm) in ((sinT, 0.0, "sinT"), (cosT, 0.5 * 3.14159265358979, "cosT")):
                if off != 0.0:
                    ts("dve", angT2[:], angT[:], off, None, ALU.add, None, ["angT"], ["angT2"])
                    src, srcn = angT2, "angT2"
                else:
                    src, srcn = angT, "angT"
                ts("dve", angQ[:], src[:], INV2PI, None, ALU.mult, None, [srcn], ["angQ"])
                cp("dve", angI[:], angQ[:], ["angQ"], ["angI"])
                cp("dve", angQ[:], angI[:], ["angI"], ["angQ"])
                stt(angQ[:], angQ[:], -6.283185307179586, src[:], ALU.mult, ALU.add, ["angQ", srcn], ["angQ"])
                ts("dve", angQ[:], angQ[:], PI_LO, -PI_LO, ALU.min, ALU.max, ["angQ"], ["angQ"])
                act(dst[:], angQ[:], AF.Sin, ["angQ"], [nm])

            if a == 0:
                ln_a(0, xt, bn, mv, rstd, lnnm)
                ln_b(0, xt)
                S.barrier()
            if a == 0:
                dump("hT", hT[:], hT_all, [P, 8, BLK], BF16)

            kslot = (a % 5) * BLK

            def rope_tile(ps, psn, t, scale, out_bf, outn, bi):
                psv = ps.rearrange("p (h f j) -> p h f j", h=4, f=2)
                t1v = t1[bi][:].rearrange("p (h f j) -> p h f j", h=4, f=2)
                t2v = t2[bi][:].rearrange("p (h f j) -> p h f j", h=4, f=2)
                ov = out_bf.rearrange("p (h f j) -> p h f j", h=4, f=2)
                cb = cosT[:, t, :].unsqueeze(1).to_broadcast([P, 8, 64])
                sb1 = sinT[:, t, :].unsqueeze(1).to_broadcast([P, 4, 64])
                stt(t1[bi][:].rearrange("p (g j) -> p g j", j=64), ps.rearrange("p (g j) -> p g j", j=64), scale, cb,
                    ALU.mult, ALU.mult, [psn, "cosT"], [("t1", bi)])
                stt(t2v[:, :, 0, :], psv[:, :, 1, :], scale, sb1, ALU.mult, ALU.mult, [psn, "sinT"], [("t2a", bi)])
                stt(t2v[:, :, 1, :], psv[:, :, 0, :], scale, sb1, ALU.mult, ALU.mult, [psn, "sinT"], [("t2b", bi)])
                tt("dve", ov[:, :, 0, :], t1v[:, :, 0, :], t2v[:, :, 0, :], ALU.subtract,
                   [("t1", bi), ("t2a", bi)], [(outn, "a")])
                tt("dve", ov[:, :, 1, :], t1v[:, :, 1, :], t2v[:, :, 1, :], ALU.add,
                   [("t1", bi), ("t2b", bi)], [(outn, "b")])

            pend = []
            ri = [0]

            def flush():
                while pend:
                    pend.pop(0)()

            def rope_item(w, wn, t, scale, dst_ap, dst_name):
                bi = ri[0] % 2
                ri[0] += 1
                ps, psn = rot()
                proj_tok(ps, psn, w, wn, lambda k, t=t: hT[:, k, t * P:(t + 1) * P], [("hT", t)])
                flush()
                rope_tile(ps, psn, t, scale, qr[bi][:], ("qr", bi), bi)

                def later():
                    for h in range(4):
                        tr(ptA[:, h * 128:(h + 1) * 128], qr[bi][:, h * 128:(h + 1) * 128], ident_b[:],
                           [(("qr", bi), "a"), (("qr", bi), "b"), "ident_b"], ["ptA"], inc=(h == 3))
                    cp("act", dst_ap, ptA[:, 0:512].rearrange("p (h j) -> p h j", h=4), ["ptA"], [dst_name])
                pend.append(later)

            w, wn = w_cur(a, CK_K)
            for t in range(4):
                rope_item(w, wn, t, 1.0, KT[:, :, kslot + t * P:kslot + (t + 1) * P], ("KT", a % 5))
            w_done()
            w, wn = w_cur(a, CK_V)
            for t in range(4):
                ps, psn = rot()
                proj_tok(ps, psn, w, wn, lambda k, t=t: hT[:, k, t * P:(t + 1) * P], [("hT", t)])
                flush()
                cp("act", V0[:, (4 * a + t) % 8, :], ps, [psn], [("V0", (4 * a + t) % 8)])
            for r in range(4):
                ps, psn = rot()
                proj_tok(ps, psn, w, wn, lambda k, r=r: hT[:, k, r:BLK:4], hT_all)
                cp("act", V1[:, (a % 5) * 4 + r, :], ps, [psn], [("V1", (a % 5) * 4 + r)])
            w_done()
            for g in range(3):
                w, wn = w_cur(a, CK_Q0 + g)
                for t in range(4):
                    rope_item(w, wn, t, 128.0 ** -0.5, QT[:, 4 * g:4 * g + 4, t * P:(t + 1) * P], ("QT", g))
                w_done()
            w, wn = w_cur(a, CK_ZA)
            for r in range(4):
                ps, psn = rot()
                proj_tok(ps, psn, w, wn, lambda k, r=r: hT[:, k, r:BLK:4], hT_all)
                flush()
                act(zs[:, r, :], ps, AF.Silu, [psn], [("zs", r)])
            w_done()
            flush()
            if a == 0:
                dump("QT", QT[:], [("QT", g) for g in range(3)], [P, 12, BLK], BF16)
                dump("KT", KT[:, :, 0:BLK], [("KT", 0)], [P, 4, BLK], BF16)
                dump("V1", V1[:, 0:4, :], [("V1", r) for r in range(4)], [P, 4, 512], BF16)

            def unit_tiles(r, h):
                tiles = []
                for j in range(0 if a > 0 else 1, 5):
                    tile_idx = 4 * a + j - 1
                    ring = tile_idx // 4 % 5
                    kc = ring * BLK + (tile_idx % 4) * P
                    tiles.append((0, KT[:, h, kc:kc + P], V0[:, tile_idx % 8, h * 128:(h + 1) * 128],
                                  ("V0", tile_idx % 8), ("KT", ring)))
                n0 = len(tiles)
                for aa in range(max(0, a - 1), a + 1):
                    ring = aa % 5
                    tiles.append((1, KT[:, h, ring * BLK + r:(ring + 1) * BLK:4], V1[:, ring * 4 + r, h * 128:(h + 1) * 128],
                                  ("V1", ring * 4 + r), ("KT", ring)))
                n1 = len(tiles) - n0
                for aa in range(max(0, a - 4), a + 1):
                    ring = aa % 5
                    tiles.append((2, KT[:, h, ring * BLK + r:(ring + 1) * BLK:4], V1[:, ring * 4 + r, h * 128:(h + 1) * 128],
                                  ("V1", ring * 4 + r), ("KT", ring)))
                n2 = len(tiles) - n0 - n1
                return tiles, n0, n1, n2

            def a_qk(r, h):
                tiles, n0, n1, n2 = unit_tiles(r, h)
                for ti, (g, kap, vap, vn, kn) in enumerate(tiles):
                    mm(ps_s[:, ti * 128:(ti + 1) * 128], QT[:, 4 * g + h, r:BLK:4], kap, True, True,
                       [("QT", g), kn], [PS_S[ti // 4]])

            def a_softmax(r, h, ub):
                tiles, n0, n1, n2 = unit_tiles(r, h)
                ncol = len(tiles) * 128
                c0 = 0
                segs = [(n0, mg0[:, r, 640 - n0 * 128:640], "mg0"),
                        (n1, mg12[:, 256 - n1 * 128:256], "mg12"),
                        (n2, mg12[:, 896 - n2 * 128:896] if a < 4 else mg12[:, 256:896], "mg12")]
                for (nseg, mk, mkn) in segs:
                    w_ = nseg * 128
                    banks = sorted(set(PS_S[c // 512] for c in range(c0, c0 + w_, 128)))
                    tt("dve", sm[ub][:, c0:c0 + w_], ps_s[:, c0:c0 + w_], mk, ALU.add, banks + [mkn], [("sm", ub, c0 // 128)])
                    c0 += w_
                smr = [("sm", ub, 0), ("sm", ub, n0), ("sm", ub, n0 + n1)]
                S.op("dve", lambda e, o=stat[:, 16 + ub:17 + ub], i=sm[ub][:, 0:ncol]: e.reduce_max(o, i, axis=AX.X),
                     reads=smr, writes=[("stat0", ub)])
                ts("dve", stat[:, 20 + ub:21 + ub], stat[:, 16 + ub:17 + ub], -1.0, None, ALU.mult, None, [("stat0", ub)], [("stat1", ub)])
                act(Pb[ub][:, 0:ncol], sm[ub][:, 0:ncol], AF.Exp, smr + [("stat1", ub)], [("Pb", ub), ("den", r % 2, h)],
                    bias=stat[:, 20 + ub:21 + ub], accum=stat[:, (r % 2) * 4 + h:(r % 2) * 4 + h + 1])

            def a_trans(r, h, ub, pb_):
                tiles, n0, n1, n2 = unit_tiles(r, h)
                nt = len(tiles)
                for ti in range(nt):
                    pt = ptA if ti < 8 else ptB
                    ptn = "ptA" if ti < 8 else "ptB"
                    o = (ti % 8) * 128
                    last = (ti == nt - 1) or (ti == 7)
                    tr(pt[:, o:o + 128], Pb[pb_][:, ti * 128:(ti + 1) * 128], ident_b[:], [("Pb", pb_), "ident_b"], [ptn], inc=last)
                nA = min(nt, 8)
                cp("act", PT[ub][:, 0:nA, :], ptA[:, 0:nA * 128].rearrange("p (t j) -> p t j", j=128), ["ptA"], [("PTa", ub)])
                if nt > 8:
                    cp("act", PT[ub][:, 8:nt, :], ptB[:, 0:(nt - 8) * 128].rearrange("p (t j) -> p t j", j=128), ["ptB"], [("PTb", ub)])

            def a_pv(r, h, ub):
                tiles, n0, n1, n2 = unit_tiles(r, h)
                nt = len(tiles)
                po, pon = PB[3 + r % 2], PBN[3 + r % 2]
                for ti, (g, kap, vap, vn, kn) in enumerate(tiles):
                    mm(po[:, h * 128:(h + 1) * 128], PT[ub][:, ti, :], vap, ti == 0, ti == nt - 1,
                       [("PTa", ub), ("PTb", ub), vn], [pon])

            def a_final(r):
                po, pon = PB[3 + r % 2], PBN[3 + r % 2]
                rb = (r % 2) * 4
                S.op("dve", lambda e, o=stat[:, 8 + rb:12 + rb], i=stat[:, rb:rb + 4]: e.reciprocal(o, i),
                     reads=[("den", r % 2, h) for h in range(4)], writes=[("rden", r % 2)])
                for h in range(4):
                    stt(og[:, h * 128:(h + 1) * 128], po[:, h * 128:(h + 1) * 128], stat[:, 8 + rb + h:9 + rb + h],
                        zs[:, r, h * 128:(h + 1) * 128], ALU.mult, ALU.mult, [pon, ("rden", r % 2), ("zs", r)], [("og", h)])

            def a_final_pe(r):
                for h in range(4):
                    tr(PB[5].bitcast(BF16)[:, h * 128:(h + 1) * 128], og[:, h * 128:(h + 1) * 128], ident_b[:], [("og", h), "ident_b"], ["PB5"],
                       inc=(h == 3))
                cp("act", ogT[:, :, r:BLK:4], PB[5].bitcast(BF16)[:, 0:512].rearrange("p (h j) -> p h j", h=4), ["PB5"], [("ogT", r)])

            if a > 0:
                S.wait_tokens(fence2)
            units = [(r, h) for r in range(4) for h in range(4)]
            S.op("pool", lambda e, o=stat[:, 0:8]: e.memset(o, 0.0), writes=[("den", rb, h) for rb in range(2) for h in range(4)])
            def a_sm(ui):
                r, h = units[ui]
                if h == 0 and r >= 2:
                    S.op("pool", lambda e, o=stat[:, (r % 2) * 4:(r % 2) * 4 + 4]: e.memset(o, 0.0),
                         writes=[("den", r % 2, hh) for hh in range(4)])
                a_softmax(r, h, ui % 3)

            a_qk(*units[0])
            a_sm(0)
            a_qk(*units[1])
            a_sm(1)
            for ui, (r, h) in enumerate(units):
                a_trans(r, h, ui % 2, ui % 3)
                if ui + 2 < len(units):
                    a_qk(*units[ui + 2])
                    a_sm(ui + 2)
                if h == 0 and r > 0:
                    a_final_pe(r - 1)
                a_pv(r, h, ui % 2)
                if h == 3:
                    a_final(r)
            a_final_pe(3)
            if a == 0:
                dump("ogT", ogT[:], [("ogT", r) for r in range(4)], [P, 4, BLK], BF16)
            if a == 0:
                print('phase ABC end', A.off)
            S.barrier()

            A.release(phase_mark)
            xmT = A.alloc("xmT", [P, 8, 516], BF16)
            xcT = A.alloc("xcT", [P, 8, BLK], BF16)
            zmT = A.alloc("zmT", [P, 8, BLK], BF16)
            om = A.alloc("om", [P, 4, D], BF16)
            qmT = A.alloc("qmT", [P, 8, BLK], BF16)
            kmT = A.alloc("kmT", [P, 8, BLK], BF16)
            cacc = [A.alloc("cacc0", [P, 512], F32), A.alloc("cacc1", [P, 512], F32)]
            vt = [A.alloc("vt0", [P, 4, 257], BF16), A.alloc("vt1", [P, 4, 257], BF16)]
            kt = [A.alloc("kt0", [P, D], BF16), A.alloc("kt1", [P, D], BF16)]
            smT = [A.alloc("smT0", [P, 4, 128], BF16), A.alloc("smT1", [P, 4, 128], BF16)]
            gsb4 = A.alloc("gsb4", [P, 32], F32)
            ge4 = A.alloc("ge4", [P, 16], F32)
            gn4 = A.alloc("gn4", [P, 16], F32)
            gv4 = A.alloc("gv4", [P, 16], F32)
            gp4 = A.alloc("gp4", [P, 48], F32)
            gM4 = A.alloc("gM4", [P, 16], F32)
            gx4 = A.alloc("gx4", [P, 48], F32)
            mx16 = A.alloc("mx16", [P, 32], F32)
            hg = [A.alloc("hg0", [P, D], F32), A.alloc("hg1", [P, D], F32)]
            hn = A.alloc("hn", [P, D], BF16)
            bn2 = A.alloc("bn2", [P, 4, 6], F32)
            mv2 = A.alloc("mv2", [P, 4, 2], F32)
            rs2 = A.alloc("rs2", [P, 4], F32)
            nmr = A.alloc("nmr", [P, 4], F32)
            dcol = A.alloc("dcol", [P, 16], F32)
            e1 = [A.alloc("e1_0", [P, 128], F32), A.alloc("e1_1", [P, 128], F32)]
            e2 = [A.alloc("e2_0", [P, 128], F32), A.alloc("e2_1", [P, 128], F32)]
            if a == 0:
                print('phase DE end', A.off)
            cp("pool", xmT[:, :, 0:3], halo[:, :, 0:3], ["halo"], ["xm_halo"])
            for c in range(2):
                w, wn = w_cur(a, CK_XM0 + c)
                for jj in range(4):
                    j = c * 4 + jj
                    bi = jj % 2
                    ps, psn = rot()
                    proj_feat(ps, psn, w, wn, jj * 128)
                    cp("act", xmT[:, j, 3:515], ps, [psn], [("xmT", j)])
                    cp("pool", halo[:, j, 0:3], xmT[:, j, 512:515], [("xmT", j)], ["halo"])
                    ca, can = cacc[bi], ("cacc", bi)
                    ts("dve", ca[:], xmT[:, j, 0:512], convw[:, j, 0:1], None, ALU.mult, None,
                       [("xmT", j), "xm_halo", "convw"], [can])
                    for tap in range(1, 4):
                        stt(ca[:], xmT[:, j, tap:tap + 512], convw[:, j, tap:tap + 1], ca[:], ALU.mult, ALU.add,
                            [("xmT", j), "xm_halo", "convw", can], [can])
                    act(xcT[:, j, :], ca[:], AF.Silu, [can, "convb"], [("xcT", j)], bias=convb[:, j:j + 1])
                w_done()
            for c in range(2):
                w, wn = w_cur(a, CK_ZM0 + c)
                for jj in range(4):
                    j = c * 4 + jj
                    bi = jj % 2
                    ps, psn = rot()
                    proj_feat(ps, psn, w, wn, jj * 128)
                    act(zmT[:, j, :], ps, AF.Silu, [psn], [("zmT", j)])
                w_done()
            for c in range(2):
                w, wn = w_cur(a, CK_OM0 + c)
                for t in range(4):
                    bi = t % 2
                    ps, psn = rot()
                    proj_tok(ps, psn, w, wn, lambda k, t=t: hT[:, k, t * P:(t + 1) * P], [("hT", t)])
                    act(om[:, t, c * 512:(c + 1) * 512], ps, AF.Sigmoid, [psn], [("om", t, c)])
                w_done()
            xc_all = [("xcT", j) for j in range(8)]
            xm_all = [("xmT", j) for j in range(8)]
            for h in range(4):
                for ec in range(2):
                    for which, (wb, wbn, dst, dn, scl) in enumerate(((wq_b, "wq_b", qmT, "qmT", 1.0), (wk_b, "wk_b", kmT, "kmT", 1.0 / 16.0))):
                        bi = (ec + which) % 2
                        ps, psn = rot()
                        for dc in range(2):
                            mm(ps, wb[:, h, dc, ec * 128:(ec + 1) * 128], xcT[:, 2 * h + dc, :], dc == 0, dc == 1,
                               [wbn, ("xcT", 2 * h + dc)], [psn])
                        if scl == 1.0:
                            cp("act", dst[:, 2 * h + ec, :], ps, [psn], [(dn, 2 * h + ec)])
                        else:
                            S.op("act", lambda e, o=dst[:, 2 * h + ec, :], i=ps, s=scl: e.mul(o, i, s), reads=[psn], writes=[(dn, 2 * h + ec)])
            if a == 0:
                dump("xcT", xcT[:], xc_all, [P, 8, BLK], BF16)
                dump("qmT", qmT[:], [("qmT", j) for j in range(8)], [P, 8, BLK], BF16)
                dump("kmT", kmT[:], [("kmT", j) for j in range(8)], [P, 8, BLK], BF16)

            ptBf = ptB[:].bitcast(F32)
            for c in range(4):
                tokc = slice(c * P, (c + 1) * P)
                tokxc = slice(3 + c * P, 3 + (c + 1) * P)
                for j in range(8):
                    mm(PB[3][:, c * 8:(c + 1) * 8], xcT[:, j, tokc], AB[:, j, :], j == 0, False, [("xcT", j), "AB"], ["PB3"], inc=False)
                for j in range(8):
                    mm(PB[3][:, c * 8:(c + 1) * 8], xmT[:, j, tokxc], AB[:, 8 + j, :], False, j == 7, [("xmT", j), "AB"], ["PB3"], inc=(j == 7))
            g4 = gsb4[:].rearrange("p (c g) -> p c g", g=8)
            tt("dve", g4, PB[3][:, 0:32].rearrange("p (c g) -> p c g", g=8), bif_bc[:].unsqueeze(1).to_broadcast([P, 4, 8]),
               ALU.add, ["PB3", "bif_bc"], ["gsb4"])
            act(ge4[:].rearrange("p (c h) -> p c h", h=4), g4[:, :, 4:8], AF.Exp, ["gsb4"], ["ge4"], scale=-1.0)
            act(gn4[:], ge4[:], AF.Ln, ["ge4", "cols"], ["gn4"], bias=one_col)
            for c in range(4):
                mm(PB[3][:, 32 + c * 4:36 + c * 4], tri_f, gn4[:, c * 4:(c + 1) * 4], True, True, ["cmat", "gn4"], ["PB3"])
                mm(PB[3][:, 48 + c * 4:52 + c * 4], ones_f, gn4[:, c * 4:(c + 1) * 4], True, True, ["cmat", "gn4"], ["PB3"])
            tt("dve", gv4[:].rearrange("p (c h) -> p c h", h=4), g4[:, :, 0:4], PB[3][:, 32:48].rearrange("p (c h) -> p c h", h=4),
               ALU.add, ["gsb4", "PB3"], ["gv4"])
            tr(PB[3][0:16, 128:256], gv4[:], ident_f, ["gv4", "cmat"], ["PB3"])
            S.op("dve", lambda e, o=mx16[0:16, 0:1], i=PB[3][0:16, 128:256]: e.reduce_max(o, i, axis=AX.X), reads=["PB3"], writes=["mx16"])
            ts("dve", mx16[0:16, 16:32], ident_f[0:16, 0:16], mx16[0:16, 0:1], None, ALU.mult, None, ["mx16", "cmat"], ["mxd16"])
            mm(PB[3][:, 64:80], ones_f[0:16, :], mx16[0:16, 16:32], True, True, ["cmat", "mxd16"], ["PB3"])
            cp("dve", gp4[:], PB[3][:, 32:80], ["PB3"], ["gp4"])
            if a > 0:
                cp("dve", mp4[:, 0:4], mp4[:, 16:20], ["mp4"], ["mp4"])
            for c in range(4):
                tt("dve", gM4[:, c * 4:(c + 1) * 4], mp4[:, c * 4:(c + 1) * 4], gp4[:, 32 + c * 4:36 + c * 4], ALU.max, ["mp4", "gp4"], ["gM4"])
                tt("dve", mp4[:, (c + 1) * 4:(c + 2) * 4], gM4[:, c * 4:(c + 1) * 4], gp4[:, 16 + c * 4:20 + c * 4], ALU.subtract,
                   ["gM4", "gp4", "mp4"], ["mp4"])
            tt("dve", gx4[:, 0:16], mp4[:, 0:16], gM4[:], ALU.subtract, ["mp4", "gM4"], ["gx4a"])
            tt("dve", gx4[:, 16:32], gv4[:], gM4[:], ALU.subtract, ["gv4", "gM4"], ["gx4b"])
            tt("dve", gx4[:, 32:48], gp4[:, 0:16], gM4[:], ALU.subtract, ["gp4", "gM4"], ["gx4c"])
            act(gx4[:], gx4[:], AF.Exp, ["gx4a", "gx4b", "gx4c"], ["gx4"])
            if a == 0:
                dump("gx4", gx4[:], ["gx4"], [P, 48])

            def e_pre(t):
                tb = t % 2
                tok = slice(t * P, (t + 1) * P)
                tokx = slice(3 + t * P, 3 + (t + 1) * P)
                p_ = gx4[:, 16 + 4 * t:20 + 4 * t]
                for hp in range(2):
                    ps, psn = PB[4 + hp], PBN[4 + hp]
                    for hh in range(2):
                        h = 2 * hp + hh
                        for dc in range(2):
                            mm(ps[:, hh * 256:(hh + 1) * 256], xmT[:, 2 * h + dc, tokx], wv_b[:, h, dc, :], dc == 0, dc == 1,
                               [("xmT", 2 * h + dc), "wv_b"], [psn], inc=(dc == 1 and hh == 1))
                    for hh in range(2):
                        h = 2 * hp + hh
                        S.op("act", lambda e, o=vt[tb][:, h, 0:256], i=ps[:, hh * 256:(hh + 1) * 256], sc=p_[:, h:h + 1]:
                             e.activation(o, i, AF.Identity, scale=sc), reads=[psn, "gx4"], writes=[("vt", tb, h)])
                cp("dve", vt[tb][:, :, 256:257], p_.unsqueeze(2), ["gx4"], [("vt1", tb)])
                for hp in range(2):
                    ps, psn = PB[4 + hp], PBN[4 + hp]
                    for hh in range(2):
                        h = 2 * hp + hh
                        for dc in range(2):
                            mm(ps[:, hh * 256:(hh + 1) * 256], xcT[:, 2 * h + dc, tok], wk_b[:, h, dc, :], dc == 0, dc == 1,
                               [("xcT", 2 * h + dc), "wk_b"], [psn], inc=(dc == 1 and hh == 1))
                    S.op("act", lambda e, o=kt[tb][:, hp * 512:(hp + 1) * 512], i=ps: e.mul(o, i, 1.0 / 16.0), reads=[psn], writes=[("kt", tb, hp)])
                for h in range(4):
                    for ec in range(2):
                        mm(PB[2][:, h * 128:(h + 1) * 128], kmT[:, 2 * h + ec, tok], qmT[:, 2 * h + ec, tok], ec == 0, ec == 1,
                           [("kmT", 2 * h + ec), ("qmT", 2 * h + ec)], ["PB2"], inc=(ec == 1 and h == 3))
                tt("dve", smT[tb][:], PB[2].rearrange("p (h j) -> p h j", h=4), triU_b[:].unsqueeze(1).to_broadcast([P, 4, 128]),
                   ALU.mult, ["PB2", "triU_b"], [("smT", tb)])

            def e_state(t):
                tb = t % 2
                tok = slice(t * P, (t + 1) * P)
                sc_ = gx4[:, 4 * t:4 * t + 4]
                thr_ = gx4[:, 32 + 4 * t:36 + 4 * t]
                hgt = hg[tb]
                for h in range(4):
                    S.op("act", lambda e, o=Cb[:, 2 * h:2 * h + 2, :], i=Cst[:, 2 * h:2 * h + 2, :], sc=sc_[:, h:h + 1]:
                         e.activation(o, i, AF.Identity, scale=sc), reads=[("Cst", h), "gx4"], writes=[("Cb", h)])
                for h in range(4):
                    for ec in range(2):
                        pc, pcn = (PB[3], "PB3") if ec == 0 else (ptBf, "ptB")
                        mm(pc[:, 0:257], kt[tb][:, h * 256 + ec * 128:h * 256 + (ec + 1) * 128], vt[tb][:, h, :], True, True,
                           [("kt", tb, h // 2), ("vt", tb, h), ("vt1", tb)], [pcn])
                        stt(Cst[:, 2 * h + ec, :], Cst[:, 2 * h + ec, :], sc_[:, h:h + 1], pc[:, 0:257], ALU.mult, ALU.add,
                            [("Cst", h), ("Cb", h), "gx4", pcn], [("Cst", h)])
                for h in range(4):
                    ps, psn = PB[h % 2], PBN[h % 2]
                    for ec in range(2):
                        mm(ps[:, 0:257], qmT[:, 2 * h + ec, tok], Cb[:, 2 * h + ec, :], ec == 0, False,
                           [("qmT", 2 * h + ec), ("Cb", h)], [psn], inc=False)
                    mm(ps[:, 0:257], smT[tb][:, h, :], vt[tb][:, h, :], False, True, [("smT", tb), ("vt", tb, h), ("vt1", tb)], [psn])
                    ts("dve", dcol[:, 4 + h:5 + h], ps[:, 256:257], -1.0, None, ALU.mult, None, [psn], [("dcolA", h)])
                    tt("dve", dcol[:, h:h + 1], ps[:, 256:257], dcol[:, 4 + h:5 + h], ALU.max, [psn, ("dcolA", h)], [("dcolB", h)])
                    tt("dve", dcol[:, 8 + h:9 + h], dcol[:, h:h + 1], thr_[:, h:h + 1], ALU.max, [("dcolB", h), "gx4"], [("dcolC", h)])
                    S.op("dve", lambda e, o=dcol[:, 12 + h:13 + h], i=dcol[:, 8 + h:9 + h]: e.reciprocal(o, i), reads=[("dcolC", h)], writes=[("dcolD", h)])
                    stt(hgt[:, h * 256:(h + 1) * 256], ps[:, 0:256], dcol[:, 12 + h:13 + h], om[:, t, h * 256:(h + 1) * 256],
                        ALU.mult, ALU.mult, [psn, ("dcolD", h), ("om", t, h // 2)], [("hg", tb, h)])
            def e_h2(t):
                tb = t % 2
                tok = slice(t * P, (t + 1) * P)
                hgt = hg[tb]
                for h in range(4):
                    S.op("dve", lambda e, o=bn2[:, h, :], i=hgt[:, h * 256:(h + 1) * 256]: e.bn_stats(o, i),
                         reads=[("hg", tb, h)], writes=[("bn2", h)])
                    S.op("dve", lambda e, o=mv2[:, h, :], i=bn2[:, h, :]: e.bn_aggr(o, i), reads=[("bn2", h)], writes=[("mv2", h)])
                mv2r = [("mv2", h) for h in range(4)]
                act(rs2[:], mv2[:, :, 1], AF.Sqrt, mv2r + ["cols"], ["rs2"], bias=eps_col)
                S.op("dve", lambda e, o=rs2[:]: e.reciprocal(o, o), reads=["rs2"], writes=["rs2"])
                stt(nmr[:], mv2[:, :, 0], -1.0, rs2[:], ALU.mult, ALU.mult, mv2r + ["rs2"], ["nmr"])
                for h in range(4):
                    act(hn[:, h * 256:(h + 1) * 256], hgt[:, h * 256:(h + 1) * 256], AF.Identity, [("hg", tb, h), "rs2", "nmr"], [("hn", h)],
                        bias=nmr[:, h:h + 1], scale=rs2[:, h:h + 1])
                if a == 0 and t == 0:
                    dump("hn", hn[:], [("hn", h) for h in range(4)], [P, D], BF16)
                for j in range(8):
                    tr(ptA[:, j * 128:(j + 1) * 128], hn[:, j * 128:(j + 1) * 128], ident_b[:], [("hn", j // 2), "ident_b"], ["ptA"],
                       inc=(j == 7))
                for j in range(8):
                    bi = j % 2
                    S.op("act", lambda e, o=e1[bi][:], i=xcT[:, j, tok], sc=skipc[:, j:j + 1]: e.activation(o, i, AF.Identity, scale=sc),
                         reads=[("xcT", j), "skipc"], writes=[("e1", bi)])
                    stt(e2[bi][:], ptA[:, j * 128:(j + 1) * 128], mhw[:, j:j + 1], e1[bi][:], ALU.mult, ALU.add,
                        ["ptA", "mhw", ("e1", bi)], [("e2", bi)])
                    tt("dve", yminT[:, j, tok], e2[bi][:], zmT[:, j, tok], ALU.mult, [("e2", bi), ("zmT", j)], [("yminT", j)])

            e_pre(0)
            e_state(0)
            for t in range(4):
                if t < 3:
                    e_pre(t + 1)
                    e_state(t + 1)
                e_h2(t)
            if a == 0:
                dump("yminT", yminT[:], [("yminT", j) for j in range(8)], [P, 8, BLK], BF16)
            S.barrier()

            A.release(phase_mark)
            gaT = A.alloc("gaT", [P, 8, BLK], BF16)
            gmT = A.alloc("gmT", [P, 8, BLK], BF16)
            mT = A.alloc("mT", [P, 8, BLK], BF16)
            u1 = [A.alloc("u1_0", [P, 512], F32), A.alloc("u1_1", [P, 512], F32)]
            xt_n = [A.alloc("xtn%d" % i, [P, D], F32) for i in range(4)]
            bn_n = A.alloc("bn_n", [P, 4, 2, 6], F32)
            mv_n = A.alloc("mv_n", [P, 4, 2], F32)
            rstd_n = A.alloc("rstd_n", [P, 4], F32)
            lnnm_n = A.alloc("lnnm_n", [P, 4], F32)
            fg_early_end = A.off
            res = A.alloc("res", [P, 4, D], F32)
            xr = [A.alloc("xr%d" % i, [P, D], F32) for i in range(4)]
            bn3 = A.alloc("bn3", [P, 4, 2, 6], F32)
            mv3 = A.alloc("mv3", [P, 4, 2], F32)
            rs3 = A.alloc("rs3", [P, 4], F32)
            nm3 = A.alloc("nm3", [P, 4], F32)
            assert b_end <= fg_early_end, (b_end, fg_early_end)
            for t in range(4):
                dma(xr[t][:], x_d[t0 + t * P:t0 + (t + 1) * P, :], [], [("xr", t)])
            if a == 0:
                print('phase FG end', A.off)
            for which, (dst, dn) in enumerate(((gaT, "gaT"), (gmT, "gmT"))):
                for c in range(2):
                    w, wn = w_cur(a, (CK_GA0 if which == 0 else CK_GM0) + c)
                    for jj in range(4):
                        j = c * 4 + jj
                        bi = jj % 2
                        ps, psn = rot()
                        proj_feat(ps, psn, w, wn, jj * 128)
                        act(dst[:, j, :], ps, AF.Sigmoid, [psn], [(dn, j)])
                    w_done()
            if a + 1 < NBLK:
                ln_a(a + 1, xt_n, bn_n, mv_n, rstd_n, lnnm_n)
            w, wn = w_cur(a, CK_PA)
            wpa = w[:].rearrange("p k n -> p (k n)").rearrange("p (k n) -> p k n", k=4)
            ogr = [("ogT", r) for r in range(4)]
            u1s = {}
            for j in range(8):
                bi = j % 2
                ps, psn = rot()
                for k in range(4):
                    mm(ps, wpa[:, k, j * 128:(j + 1) * 128], ogT[:, k, :], k == 0, k == 3, [wn] + ogr, [psn])
                tt("dve", mT[:, j, :], ps, gaT[:, j, :], ALU.mult, [psn, ("gaT", j)], [("mT", j)])
            w_done()
            for c in range(2):
                w, wn = w_cur(a, CK_PM0 + c)
                for jj in range(4):
                    j = c * 4 + jj
                    bi = jj % 2
                    ps, psn = rot()
                    for k in range(8):
                        mm(ps, w[:, k, jj * 128:(jj + 1) * 128], yminT[:, k, :], k == 0, k == 7,
                           [wn] + [("yminT", kk) for kk in range(8)], [psn])
                    tt("dve", u1[bi][:], ps, gmT[:, j, :], ALU.mult, [psn, ("gmT", j)], [("u1", bi)])
                    tt("dve", mT[:, j, :], u1[bi][:], mT[:, j, :], ALU.add, [("u1", bi), ("mT", j)], [("mT", j)])
                w_done()
            if a == 0:
                dump("mT", mT[:], [("mT", j) for j in range(8)], [P, 8, BLK], BF16)
            alpha = 2.0 ** 0.25
            for c in range(2):
                w, wn = w_cur(a, CK_OUT0 + c)
                for t in range(4):
                    bi = t % 2
                    ps, psn = rot()
                    for k in range(8):
                        mm(ps, mT[:, k, t * P:(t + 1) * P], w[:, k, :], k == 0, k == 7, [wn] + [("mT", kk) for kk in range(8)], [psn])
                    tt("dve", res[:, t, c * 512:(c + 1) * 512], ps, gate_bc[:, c * 512:(c + 1) * 512], ALU.mult,
                       [psn, "gate_bc"], [("res", t, c)])
                w_done()
            if a + 1 < NBLK:
                ln_b(a + 1, xt_n)
            fence1 = S.snapshot()
            for t in range(4):
                xs, xn = xr[t], ("xr", t)
                stt(xs[:], xs[:], alpha, res[:, t, :], ALU.mult, ALU.add, [xn, ("res", t, 0), ("res", t, 1)], [xn])
            for t in range(4):
                xs, xn = xr[t], ("xr", t)
                for hf in range(2):
                    S.op("dve", lambda e, o=bn3[:, t, hf, :], i=xs[:, hf * 512:(hf + 1) * 512]: e.bn_stats(o, i),
                         reads=[xn], writes=[("bn3", t)])
                S.op("dve", lambda e, o=mv3[:, t, :], i=bn3[:, t].rearrange("p a b -> p (a b)"): e.bn_aggr(o, i),
                     reads=[("bn3", t)], writes=[("mv3", t)])
            mv3r = [("mv3", t) for t in range(4)]
            act(rs3[:], mv3[:, :, 1], AF.Sqrt, mv3r + ["cols"], ["rs3"], bias=eps_col)
            S.op("dve", lambda e, o=rs3[:]: e.reciprocal(o, o), reads=["rs3"], writes=["rs3"])
            stt(nm3[:], mv3[:, :, 0], -1.0, rs3[:], ALU.mult, ALU.mult, mv3r + ["rs3"], ["nm3"])
            for t in range(4):
                xs, xn = xr[t], ("xr", t)
                act(xs[:], xs[:], AF.Identity, [xn, "rs3", "nm3"], [xn], bias=nm3[:, t:t + 1], scale=rs3[:, t:t + 1])
            for t in range(4):
                xs, xn = xr[t], ("xr", t)
                tt("dve", xs[:], xs[:], lng_bc[:], ALU.mult, [xn, "lng_bc"], [xn])
                tt("dve", xs[:], xs[:], lnb_bc[:], ALU.add, [xn, "lnb_bc"], [xn])
                dma(y_d[t0 + t * P:t0 + (t + 1) * P, :], xs[:], [xn], [])
            fence2 = S.snapshot()

        S.finish()
        S.emit()
    print("op counts", S.cnt, S.dma_cnt)
    print("SBUF arena peak", A.peak, "of", SBUF_TOP, "persistent end", phase_mark)
    return nc, dumps


def _consts():
    i = np.arange(128)[:, None]
    p = np.arange(128)[None, :]
    mg0 = np.zeros((128, 4, 5, 128), np.float32)
    for r in range(4):
        for j in range(5):
            d = (4 * i + r) - (128 * (j - 1) + p)
            mg0[:, r, j, :] = np.where((d >= 0) & (d <= 128), 0.0, NEG)
    mg0 = mg0.reshape(128, 4, 640)
    g1_prev = np.where(p >= i, 0.0, NEG)
    g1_own = np.where(p <= i, 0.0, NEG)
    same = ((i - p) % 4 == 0)
    g2_far = np.where(same & (p >= i), 0.0, NEG)
    g2_mid = np.where(same, 0.0, NEG)
    g2_own = np.where(same & (p <= i), 0.0, NEG)
    mg12 = np.concatenate([g1_prev, g1_own, g2_far, g2_mid, g2_mid, g2_mid, g2_own], axis=1).astype(np.float32)
    cmat = np.zeros((128, 4, 128), np.float32)
    cmat[:, 0, :] = np.eye(128)
    cmat[:, 1, :] = 1.0
    cmat[:, 2, :] = (i <= p)
    invf = np.power(np.float32(10000.0), -np.arange(64, dtype=np.float32) / np.float32(64)).astype(np.float32)
    invf = np.broadcast_to(invf[None, :], (128, 64)).copy()
    return mg0, mg12, cmat, invf


def make_in_maps(inputs, S_LEN=4096, n_cores=8):
    f = lambda a: np.ascontiguousarray(np.asarray(a, dtype=np.float32))
    x = f(inputs["x"])
    c = f(inputs["c"])
    pos = np.asarray(inputs["positions"]).astype(np.int32)
    mg0, mg12, cmat, invf = _consts()
    col = lambda v: np.ascontiguousarray(v.reshape(-1, 128).T)
    shared = {
        "w_ada": f(inputs["w_ada"][0]),
        "b_ada_col": col(f(inputs["b_ada"][0])),
        "b_ada": f(inputs["b_ada"][0]).reshape(1, -1),
        "w_in": f(inputs["w_in"][0]),
        "convw_col": np.ascontiguousarray(f(inputs["conv_w"][0]).T.reshape(8, 128, 4).transpose(1, 0, 2)),
        "convb_col": col(f(inputs["conv_b"][0])),
        "w_qm": f(inputs["w_qm"][0]), "w_km": f(inputs["w_km"][0]), "w_vm": f(inputs["w_vm"][0]),
        "w_qmT": np.ascontiguousarray(f(inputs["w_qm"][0]).transpose(0, 2, 1)),
        "w_kmT": np.ascontiguousarray(f(inputs["w_km"][0]).transpose(0, 2, 1)),
        "w_vmT": np.ascontiguousarray(f(inputs["w_vm"][0]).transpose(0, 2, 1)),
        "w_if": f(inputs["w_if"][0]),
        "b_if": f(inputs["b_if"][0]).reshape(1, 8),
        "mhw_col": col(f(inputs["mh_norm_w"][0])),
        "skip_col": col(f(inputs["skip_m"][0])),
        "w_pa": f(inputs["w_pa"][0]), "w_pm": f(inputs["w_pm"][0]), "w_out": f(inputs["w_out"][0]),
        "ln_g": f(inputs["ln_g"][0]).reshape(1, -1), "ln_b": f(inputs["ln_b"][0]).reshape(1, -1),
        "invf": invf, "mg0": mg0, "mg12": mg12, "cmat": cmat,
    }
    maps = []
    for b in range(n_cores):
        m = dict(shared)
        m["x"] = np.ascontiguousarray(x[b, :S_LEN])
        m["cT"] = col(c[b])
        m["posT"] = np.ascontiguousarray(pos[b, :S_LEN].reshape(-1, 128).T)
        maps.append(m)
    return maps


_NC_CACHE = {}


def kernel(**inputs):
    if "nc" not in _NC_CACHE:
        _NC_CACHE["nc"] = build_nc(4096, dbg=False)[0]
    nc = _NC_CACHE["nc"]
    maps = make_in_maps(inputs, 4096, 8)
    res = run_bass_kernel_spmd(nc, maps, core_ids=list(range(8)))
    out = np.stack([np.asarray(r["y"], dtype=np.float32) for r in res.results], axis=0)
    return out
```
